# Optimizing a Trainium2 kernel written in Bass

```python
import math
import jax, jax.numpy as jnp
from jax import lax
import numpy as np

D_MODEL = 1024
BATCH = 32
SEQ = 2048
DEPTH = 2

N_BRANCHES = 4
BRANCH_WIDTH = D_MODEL // N_BRANCHES
MOBA_HEADS = 4
MOBA_HEAD_DIM = BRANCH_WIDTH // MOBA_HEADS
MOBA_BLOCK = 256
MOBA_TOPK = 3
Q_BLOCK = 128
CONV_WIDTH = 3
POOL_WINDOWS = (2, 4, 8, 16)
POOL_GROUP = BRANCH_WIDTH // len(POOL_WINDOWS)
DIFF_HEADS = 4
DIFF_V_DIM = BRANCH_WIDTH // DIFF_HEADS
DIFF_QK_DIM = DIFF_V_DIM // 2
REL_BUCKETS = 32
REL_MAX_DIST = 128
N_ATTN_HEADS = MOBA_HEADS + DIFF_HEADS
N_MIX_SLICES = 10
W_IN_WIDTH = N_MIX_SLICES * BRANCH_WIDTH + N_BRANCHES * D_MODEL
D_FF_DENSE = 2816
N_EXPERTS = 8
TOP_K_EXPERTS = 2
D_FF_EXPERT = 3584
N_DENSE = (DEPTH + 1) // 2
N_MOE = DEPTH // 2
RMS_EPS = 1e-6
SUBLN_EPS = 1e-5
NEG_INF = -1e30

kernel_name = "hybrid_moba_conv_pool_diffattn_moe"


def rmsnorm(x, g, eps=RMS_EPS):
    xf = x.astype(jnp.float32)
    r = lax.rsqrt(jnp.mean(xf * xf, axis=-1, keepdims=True) + eps)
    return (xf * r).astype(x.dtype) * g


def rel_bucket(dist):
    n = jnp.maximum(dist, 0)
    max_exact = REL_BUCKETS // 2
    nf = jnp.maximum(n, max_exact).astype(jnp.float32)
    large = max_exact + (jnp.log(nf / max_exact) / math.log(REL_MAX_DIST / max_exact)
                         * (REL_BUCKETS - max_exact)).astype(jnp.int32)
    large = jnp.minimum(large, REL_BUCKETS - 1)
    return jnp.where(n < max_exact, n, large)


def moba_attention(q, k, v, bias_tab):
    _, H, S, dh = q.shape
    n_blk = -(-S // MOBA_BLOCK)
    s_pad = n_blk * MOBA_BLOCK
    topk = min(MOBA_TOPK, n_blk)
    n_qc = s_pad // Q_BLOCK
    scale = dh ** -0.5
    pad = ((0, 0), (0, s_pad - S), (0, 0))
    head_ix = jnp.arange(H)[:, None, None, None]

    def per_seq(args):
        qb, kb, vb = args
        qb = jnp.pad(qb, pad) * scale
        k_blocks = jnp.pad(kb, pad).reshape(H, n_blk, MOBA_BLOCK, dh)
        v_blocks = jnp.pad(vb, pad).reshape(H, n_blk, MOBA_BLOCK, dh)
        k_mean = jnp.mean(k_blocks.astype(jnp.float32), axis=2).astype(kb.dtype)
        q_chunks = qb.reshape(H, n_qc, Q_BLOCK, dh).transpose(1, 0, 2, 3)

        def per_chunk(args2):
            qc, ci = args2
            t_pos = ci * Q_BLOCK + jnp.arange(Q_BLOCK)
            own = (ci * Q_BLOCK) // MOBA_BLOCK
            gate = jnp.einsum('hqd,hnd->hqn', qc, k_mean)
            gate = jnp.where(jnp.arange(n_blk) < own, gate, NEG_INF)
            _, sel = lax.top_k(gate, topk)
            valid = jnp.arange(topk) < own
            k_sel = jax.vmap(lambda kbh, ix: kbh[ix])(k_blocks, sel)
            v_sel = jax.vmap(lambda vbh, ix: vbh[ix])(v_blocks, sel)
            key_pos_sel = sel[..., None] * MOBA_BLOCK + jnp.arange(MOBA_BLOCK)
            bias_sel = bias_tab[head_ix, rel_bucket(t_pos[None, :, None, None] - key_pos_sel)]
            logit_sel = jnp.einsum('hqd,hqnkd->hqnk', qc, k_sel) + bias_sel
            logit_sel = jnp.where(valid[:, None], logit_sel, NEG_INF)
            k_own = lax.dynamic_index_in_dim(k_blocks, own, axis=1, keepdims=False)
            v_own = lax.dynamic_index_in_dim(v_blocks, own, axis=1, keepdims=False)
            rel_own = t_pos[:, None] - (own * MOBA_BLOCK + jnp.arange(MOBA_BLOCK))[None, :]
            logit_own = jnp.einsum('hqd,hkd->hqk', qc, k_own) + bias_tab[:, rel_bucket(rel_own)]
            logit_own = jnp.where(rel_own >= 0, logit_own, NEG_INF)
            logits = jnp.concatenate(
                [logit_sel.reshape(H, Q_BLOCK, topk * MOBA_BLOCK), logit_own], axis=-1)
            p = jax.nn.softmax(logits.astype(jnp.float32), axis=-1).astype(vb.dtype)
            p_sel = p[..., :topk * MOBA_BLOCK].reshape(H, Q_BLOCK, topk, MOBA_BLOCK)
            p_own = p[..., topk * MOBA_BLOCK:]
            return (jnp.einsum('hqnk,hqnkd->hqd', p_sel, v_sel)
                    + jnp.einsum('hqk,hkd->hqd', p_own, v_own))

        out = lax.map(per_chunk, (q_chunks, jnp.arange(n_qc)))
        return out.transpose(1, 0, 2, 3).reshape(H, s_pad, dh)[:, :S]

    return lax.map(per_seq, (q, k, v))


def diff_attention(q, k, v, bias_tab, lam, subln_g, lam_init):
    B_, H, _, S, dq = q.shape
    dv = v.shape[-1]
    n_qc = S // Q_BLOCK
    q = q * dq ** -0.5
    q_chunks = q.reshape(B_, H, 2, n_qc, Q_BLOCK, dq).transpose(3, 0, 1, 2, 4, 5)
    key_pos = jnp.arange(S)

    def per_chunk(args):
        qc, ci = args
        t_pos = ci * Q_BLOCK + jnp.arange(Q_BLOCK)
        rel = t_pos[:, None] - key_pos[None, :]
        bias = bias_tab[:, rel_bucket(rel)]
        logits = jnp.einsum('bhmqd,bhmsd->bhmqs', qc, k) + bias[None, :, None]
        logits = jnp.where(rel >= 0, logits, NEG_INF).astype(jnp.float32)
        p = jax.nn.softmax(logits, axis=-1)
        a = (p[:, :, 0] - lam * p[:, :, 1]).astype(v.dtype)
        return jnp.einsum('bhqs,bhsd->bhqd', a, v)

    o = lax.map(per_chunk, (q_chunks, jnp.arange(n_qc)))
    o = o.transpose(1, 2, 0, 3, 4).reshape(B_, H, S, dv)
    return rmsnorm(o, subln_g, SUBLN_EPS) * (1.0 - lam_init)


def short_conv(xt, b_gate, c_gate, w_conv):
    S = xt.shape[1]
    u = c_gate * xt
    up = jnp.pad(u, ((0, 0), (CONV_WIDTH - 1, 0), (0, 0)))
    conv = w_conv[0] * up[:, 0:S]
    for i in range(1, CONV_WIDTH):
        conv = conv + w_conv[i] * up[:, i:i + S]
    return b_gate * conv


def multiscale_pool(u, w_grp, scale):
    B_, S, _ = u.shape
    ug = u.reshape(B_, S, len(POOL_WINDOWS), POOL_GROUP)
    cs = jnp.cumsum(ug.astype(jnp.float32), axis=1)
    outs = []
    for g, w in enumerate(POOL_WINDOWS):
        c = cs[:, :, g]
        lag = jnp.pad(c, ((0, 0), (w, 0), (0, 0)))[:, :S]
        cnt = jnp.minimum(jnp.arange(1, S + 1), w).astype(jnp.float32)[None, :, None]
        outs.append((c - lag) / cnt)
    pooled = jnp.stack(outs, axis=2).astype(u.dtype) - ug
    y = jnp.einsum('bsgc,gcd->bsgd', pooled, w_grp).reshape(B_, S, BRANCH_WIDTH)
    return y * scale


def token_mixers(h, w_in, conv_w, pool_w, pool_scale, diff_lam, subln_g, branch_proj, w_out,
                 bias_table, layer_idx):
    B_, S, _ = h.shape
    u = h @ w_in
    split_pts = [BRANCH_WIDTH * i for i in range(1, N_MIX_SLICES + 1)]
    qa, ka, va, xb, bb, cb, pc, qd, kd, vd, gates = jnp.split(u, split_pts, axis=-1)

    def heads_a(t):
        return t.reshape(B_, S, MOBA_HEADS, MOBA_HEAD_DIM).transpose(0, 2, 1, 3)

    y_a = moba_attention(heads_a(qa), heads_a(ka), heads_a(va), bias_table[:, :MOBA_HEADS].T)
    y_a = y_a.transpose(0, 2, 1, 3).reshape(B_, S, BRANCH_WIDTH)

    y_b = short_conv(xb, bb, cb, conv_w)
    y_c = multiscale_pool(pc, pool_w, pool_scale)

    lam_init = 0.8 - 0.6 * math.exp(-0.3 * layer_idx)
    lam = (jnp.exp(jnp.sum(diff_lam[0] * diff_lam[1]).astype(jnp.float32))
           - jnp.exp(jnp.sum(diff_lam[2] * diff_lam[3]).astype(jnp.float32)) + lam_init)
    q_d = qd.reshape(B_, S, DIFF_HEADS, 2, DIFF_QK_DIM).transpose(0, 2, 3, 1, 4)
    k_d = kd.reshape(B_, S, DIFF_HEADS, 2, DIFF_QK_DIM).transpose(0, 2, 3, 1, 4)
    v_d = vd.reshape(B_, S, DIFF_HEADS, DIFF_V_DIM).transpose(0, 2, 1, 3)
    y_d = diff_attention(q_d, k_d, v_d, bias_table[:, MOBA_HEADS:].T, lam, subln_g, lam_init)
    y_d = y_d.transpose(0, 2, 1, 3).reshape(B_, S, BRANCH_WIDTH)

    g = jax.nn.sigmoid(gates.reshape(B_, S, N_BRANCHES, D_MODEL))
    merged = g[:, :, 0] * (y_a @ branch_proj[0])
    merged = merged + g[:, :, 1] * (y_b @ branch_proj[1])
    merged = merged + g[:, :, 2] * (y_c @ branch_proj[2])
    merged = merged + g[:, :, 3] * (y_d @ branch_proj[3])
    return merged @ w_out


def swiglu(h, w1, w3, w2):
    return (jax.nn.silu(h @ w1) * (h @ w3)) @ w2


def moe_swiglu(h, router, w1, w3, w2):
    logits = (h @ router).astype(jnp.float32)
    top_v, top_i = lax.top_k(logits, TOP_K_EXPERTS)
    top_w = jax.nn.softmax(top_v, axis=-1)
    out = jnp.zeros_like(h)
    for e in range(N_EXPERTS):
        gate_e = jnp.sum(jnp.where(top_i == e, top_w, 0.0), axis=-1, keepdims=True).astype(h.dtype)
        out = out + gate_e * swiglu(h, w1[e], w3[e], w2[e])
    return out


def setup_inputs(seed: int = 0) -> dict:
    key = jax.random.key(seed)
    ks = jax.random.split(key, 20)

    def nrm(k, shape, scale):
        return jax.random.normal(k, shape, jnp.float32) * scale

    return {
        "x": nrm(ks[0], (BATCH, SEQ, D_MODEL), 1.0),
        "bias_table": nrm(ks[1], (REL_BUCKETS, N_ATTN_HEADS), 0.5),
        "mix_norm_g": 1.0 + nrm(ks[2], (DEPTH, D_MODEL), 0.05),
        "w_in": nrm(ks[3], (DEPTH, D_MODEL, W_IN_WIDTH), D_MODEL ** -0.5),
        "conv_w": nrm(ks[4], (DEPTH, CONV_WIDTH, BRANCH_WIDTH), CONV_WIDTH ** -0.5),
        "pool_w": nrm(ks[5], (DEPTH, len(POOL_WINDOWS), POOL_GROUP, POOL_GROUP), POOL_GROUP ** -0.5),
        "pool_scale": 1.0 + nrm(ks[6], (DEPTH, BRANCH_WIDTH), 0.1),
        "diff_lambda": nrm(ks[7], (DEPTH, 4, DIFF_QK_DIM), 0.1),
        "diff_subln_g": 1.0 + nrm(ks[8], (DEPTH, DIFF_V_DIM), 0.05),
        "branch_proj": nrm(ks[9], (DEPTH, N_BRANCHES, BRANCH_WIDTH, D_MODEL), BRANCH_WIDTH ** -0.5),
        "w_out": nrm(ks[10], (DEPTH, D_MODEL, D_MODEL), D_MODEL ** -0.5),
        "ffn_norm_g": 1.0 + nrm(ks[11], (DEPTH, D_MODEL), 0.05),
        "dense_w1": nrm(ks[12], (N_DENSE, D_MODEL, D_FF_DENSE), D_MODEL ** -0.5),
        "dense_w3": nrm(ks[13], (N_DENSE, D_MODEL, D_FF_DENSE), D_MODEL ** -0.5),
        "dense_w2": nrm(ks[14], (N_DENSE, D_FF_DENSE, D_MODEL), D_FF_DENSE ** -0.5),
        "moe_router": nrm(ks[15], (N_MOE, D_MODEL, N_EXPERTS), D_MODEL ** -0.5),
        "moe_w1": nrm(ks[16], (N_MOE, N_EXPERTS, D_MODEL, D_FF_EXPERT), D_MODEL ** -0.5),
        "moe_w3": nrm(ks[17], (N_MOE, N_EXPERTS, D_MODEL, D_FF_EXPERT), D_MODEL ** -0.5),
        "moe_w2": nrm(ks[18], (N_MOE, N_EXPERTS, D_FF_EXPERT, D_MODEL), D_FF_EXPERT ** -0.5),
        "final_norm_g": 1.0 + nrm(ks[19], (D_MODEL,), 0.05),
    }


def reference(x, bias_table, mix_norm_g, w_in, conv_w, pool_w, pool_scale, diff_lambda,
              diff_subln_g, branch_proj, w_out, ffn_norm_g, dense_w1, dense_w3, dense_w2,
              moe_router, moe_w1, moe_w3, moe_w2, final_norm_g):
    for i in range(DEPTH):
        h = rmsnorm(x, mix_norm_g[i])
        x = x + token_mixers(h, w_in[i], conv_w[i], pool_w[i], pool_scale[i], diff_lambda[i],
                             diff_subln_g[i], branch_proj[i], w_out[i], bias_table, i)
        h = rmsnorm(x, ffn_norm_g[i])
        if i % 2 == 0:
            j = i // 2
            x = x + swiglu(h, dense_w1[j], dense_w3[j], dense_w2[j])
        else:
            j = i // 2
            x = x + moe_swiglu(h, moe_router[j], moe_w1[j], moe_w3[j], moe_w2[j])
    return rmsnorm(x, final_norm_g)
```

```python
import math
from contextlib import ExitStack

import numpy as np
import ml_dtypes

import concourse.bass as bass
import concourse.mybir as mybir
from concourse.bass_utils import run_bass_kernel_spmd

F32 = mybir.dt.float32
BF16 = mybir.dt.bfloat16
ALU = mybir.AluOpType
AF = mybir.ActivationFunctionType
AX = mybir.AxisListType

D = 1024
S = 2048
NCORES = 8
NSEQ = 4
NEG = -30000.0
DFF_D = 2816
DFF_E = 3584
NS_STAGE = 2
NW_RING = 4

C_MIXG = 0
C_FFNG = 16
C_FING = 32
C_CONV = 40
C_PSC = 52
C_SUBG = 56
C_FAR = 58
NCOL = 66


class Ctx:
    ENG = ['pe', 'act', 'dve', 'pool', 'sp']

    def __init__(self, nc, es):
        self.nc = nc
        self.es = es
        self.sem = {e: es.enter_context(nc.semaphore('s_' + e)) for e in self.ENG}
        self.cnt = {e: 0 for e in self.ENG}
        self.ops = {e: [] for e in self.ENG}
        self.waited = {}
        self.lastw = {}
        self.readers = {}
        self.dma_sems = {}

    def dsem(self, name):
        if name not in self.dma_sems:
            self.dma_sems[name] = [self.es.enter_context(self.nc.semaphore('d_' + name)), 0]
        return self.dma_sems[name]

    def _need(self, eng, waits, dep):
        if dep is None:
            return
        sem, val, src = dep
        if src == 'pe' and eng == 'pe':
            return
        k = (eng, id(sem))
        if self.waited.get(k, 0) >= val:
            return
        self.waited[k] = val
        waits.append((sem, val))

    def op(self, eng, fn, reads=(), writes=(), dma=None):
        waits = []
        for key in reads:
            self._need(eng, waits, self.lastw.get(key))
        for key in writes:
            self._need(eng, waits, self.lastw.get(key))
            for d in self.readers.get(key, {}).values():
                self._need(eng, waits, d)
        if dma is None:
            self.cnt[eng] += 1
            tok = (self.sem[eng], self.cnt[eng], eng)
            inc = (self.sem[eng], 1)
        else:
            ds = self.dsem(dma)
            ds[1] += 16
            tok = (ds[0], ds[1], 'dma:' + dma)
            inc = (ds[0], 16)
        for key in writes:
            self.lastw[key] = tok
            self.readers[key] = {}
        for key in reads:
            if key not in writes:
                self.readers.setdefault(key, {})[id(tok[0])] = tok

        def run(e, waits=waits, fn=fn, inc=inc):
            for (s, v) in waits:
                e.wait_ge(s, v)
            fn(e).then_inc(inc[0], inc[1])
        self.ops[eng].append(run)
        return tok

    def final_wait(self, eng, toks):
        waits = []
        best = {}
        for t in toks:
            if id(t[0]) not in best or best[id(t[0])][1] < t[1]:
                best[id(t[0])] = t
        for t in best.values():
            self._need(eng, waits, t)

        def run(e, waits=waits):
            for (s, v) in waits:
                e.wait_ge(s, v)
        self.ops[eng].append(run)

    def emit(self):
        nc = self.nc
        ops = self.ops
        with nc.Block() as blk:
            @blk.tensor
            def _(e):
                for r in ops['pe']:
                    r(e)

            @blk.scalar
            def _(e):
                for r in ops['act']:
                    r(e)

            @blk.vector
            def _(e):
                for r in ops['dve']:
                    r(e)

            @blk.gpsimd
            def _(e):
                for r in ops['pool']:
                    r(e)

            @blk.sync
            def _(e):
                for r in ops['sp']:
                    r(e)


def split_parts(n, mx):
    k = -(-n // mx)
    base = n // k
    rem = n % k
    out = []
    s = 0
    for i in range(k):
        sz = base + (1 if i < rem else 0)
        out.append((s, sz))
        s += sz
    return out


def build(nseq=NSEQ, stop=None):
    nc = bass.Bass("TRN2", target_bir_lowering=False)

    def din(name, shape, dt=F32):
        return nc.dram_tensor(name, list(shape), dt, kind="ExternalInput").ap()

    xT_d = din('xT', [nseq, 8, 128, S])
    win_d = din('w_in', [2, 52, 128, 1024])
    bp_d = din('bp', [2, 4, 8, 128, 256])
    wout_d = din('w_out', [2, 8, 128, 1024])
    dw1_d = din('dw1', [22, 128, 1024])
    dw3_d = din('dw3', [22, 128, 1024])
    dw2_d = din('dw2', [8, 128, 22 * 128])
    mw1_d = din('mw1', [8, 28, 128, 1024])
    mw3_d = din('mw3', [8, 28, 128, 1024])
    mw2_d = din('mw2', [8, 8, 128, 28 * 128])
    router_d = din('router', [128, 64])
    cols_d = din('cols', [128, NCOL])
    bias_d = din('biasT', [8, 128, 256])
    cmask_d = din('cmask', [128, 128])
    ident_d = din('ident', [128, 128])
    bones_d = din('bones', [128, 128])
    rc_d = din('rc', [128, 32])
    poolbd_d = din('poolbd', [2, 128, 256])
    dlam_d = din('dlam', [2, 128, 128])
    kblk_d = din('kblk', [8, S], BF16)
    out_d = nc.dram_tensor('outT', [nseq, 8, 128, S], F32, kind="ExternalOutput").ap()

    def dscr(name, shape):
        return nc.dram_tensor(name, list(shape), BF16, kind="Internal").ap()
    FAM = {
        'win': (win_d, dscr('sc_win', [2, 52, 128, 1024])),
        'bp': (bp_d, dscr('sc_bp', [2, 4, 8, 128, 256])),
        'wout': (wout_d, dscr('sc_wout', [2, 8, 128, 1024])),
        'dw1': (dw1_d, dscr('sc_dw1', [22, 128, 1024])),
        'dw3': (dw3_d, dscr('sc_dw3', [22, 128, 1024])),
        'dw2': (dw2_d, dscr('sc_dw2', [8, 128, 22 * 128])),
        'mw1': (mw1_d, dscr('sc_mw1', [8, 28, 128, 1024])),
        'mw3': (mw3_d, dscr('sc_mw3', [8, 28, 128, 1024])),
        'mw2': (mw2_d, dscr('sc_mw2', [8, 8, 128, 28 * 128])),
    }

    with ExitStack() as es:
        c = Ctx(nc, es)

        def sb(name, shape, dt):
            return es.enter_context(nc.sbuf_tensor(name, list(shape), dt))

        xT = sb('xTs', [128, 8, S], F32)
        hT = sb('hTs', [128, 8, S], BF16)
        arena = sb('arena', [128, 16 * S], BF16)
        arena32 = arena.bitcast(F32)
        stage = sb('stage', [128, NS_STAGE, 1024], F32)
        wbf = sb('wbf', [128, NW_RING, 1024], BF16)
        vaug = sb('vaug', [128, 16, 192], BF16)
        biasT = sb('biasTs', [128, 2, 256], F32)
        Ebuf = sb('Ebuf', [128, 4, 512], BF16)
        tmpn = sb('tmpn', [128, 2, 256], F32)
        sig = sb('sig', [128, 2, 512], F32)
        rden = sb('rden', [128, 2, 512], F32)
        cols = sb('colss', [128, NCOL], F32)
        ident = sb('idents', [128, 128], F32)
        ones32 = sb('ones32', [128, 128], F32)
        bones = sb('boness', [128, 128], F32)
        cmask = sb('cmasks', [128, 128], F32)
        rc = sb('rcs', [128, 32], F32)
        poolbd = sb('poolbds', [128, 2, 256], BF16)
        dlam = sb('dlams', [128, 2, 128], F32)
        lamw = sb('lamw', [128, 16], F32)
        ksum = sb('ksum', [128, 8], F32)
        gsb = sb('gsb', [128, 2, 8, 8], F32)
        gm = sb('gm', [128, 2, 8, 8], F32)
        mbt = sb('mbt', [128, 2, 8, 8], F32)
        m8 = sb('m8', [128, 16, 8], F32)
        router32 = sb('router32', [128, 64], F32)
        lg = sb('lg', [128, 16, 8], F32)
        ex = sb('ex', [128, 16, 8], F32)
        gtok = sb('gtok', [128, 16, 8], F32)
        smallc = sb('smallc', [128, 64], F32)
        grep = dlam
        ps = [es.enter_context(nc.psum_tensor('ps%d' % i, [128, 512], F32)) for i in range(8)]
        PK = ['ps%d' % i for i in range(8)]

        def ct(i):
            return arena[:, i * S:(i + 1) * S]

        def ctk(i):
            return 'ct%d' % i

        def sc32(k):
            return arena32[:, k * S:(k + 1) * S]

        def sck(k):
            return [ctk(2 * k), ctk(2 * k + 1)]

        bc = sc32(7)
        BCK = sck(7)

        def col(i):
            return cols[:, i:i + 1]

        def dma_in(dst, src, key, sem=None):
            return c.op('sp', lambda e: e.dma_start(out=dst, in_=src), writes=[key], dma=(sem or key))

        def mm_group(out, pskey, pairs, rkeys):
            def fn(e, pairs=pairs, out=out):
                n = len(pairs)
                ins = None
                for i, (l, r) in enumerate(pairs):
                    ins = e.matmul(out, l, r, start=(i == 0), stop=(i == n - 1))
                return ins
            return c.op('pe', fn, reads=rkeys, writes=[pskey])

        def mm1(out, pskey, l, r, start, stop, rkeys):
            return c.op('pe', lambda e: e.matmul(out, l, r, start=start, stop=stop), reads=rkeys, writes=[pskey])

        wstate = {'s': 0, 'w': 0}
        wcached = set()

        def flush_store():
            p = wstate.pop('pending', None)
            if p is not None:
                scr, wdst, wk, ckey, wi = p
                c.op('sp', lambda e: e.dma_start(out=scr, in_=wdst), reads=[wk], writes=[ckey], dma='wst%d' % wi)

        def wload(fam, idx, kc, cs=None):
            f32d, bfd = FAM[fam]
            src = f32d[idx]
            scr = bfd[idx]
            if cs is not None:
                src = src[:, cs]
                scr = scr[:, cs]
            wi = wstate['w'] % NW_RING
            wstate['w'] += 1
            wk = 'wbf%d' % wi
            wdst = wbf[:, wi, 0:kc * 128]
            ckey = 'wsc:%s:%s:%s' % (fam, idx, cs)
            if ckey in wcached:
                flush_store()
                c.op('sp', lambda e: e.dma_start(out=wdst, in_=scr), reads=[ckey], writes=[wk], dma='wld%d' % wi)
            else:
                wcached.add(ckey)
                si = wstate['s'] % NS_STAGE
                wstate['s'] += 1
                sk = 'stg%d' % si
                sdst = stage[:, si, 0:kc * 128]
                c.op('sp', lambda e: e.dma_start(out=sdst, in_=src), writes=[sk], dma=sk)
                flush_store()
                c.op('pool', lambda e: e.tensor_copy(wdst, sdst), reads=[sk], writes=[wk])
                wstate['pending'] = (scr, wdst, wk, ckey, wi)
            return wbf[:, wi, 0:kc * 128].rearrange("p (k n) -> p k n", n=128), wk

        rot = {}

        def nxt(name, n):
            v = rot.get(name, 0)
            rot[name] = v + 1
            return v % n

        dma_in(cols[:], cols_d, 'cols')
        dma_in(ident[:], ident_d, 'ident')
        dma_in(bones[:], bones_d, 'bones')
        dma_in(cmask[:], cmask_d, 'cmask')
        dma_in(rc[:], rc_d, 'rc')
        dma_in(router32[:], router_d, 'router32')
        dma_in(dlam[:, 0, :], dlam_d[0], 'grep0')
        dma_in(dlam[:, 1, :], dlam_d[1], 'grep1')
        for l in range(2):
            dma_in(stage[:, 0, 0:256], poolbd_d[l], 'stg0', sem='stg0')
            c.op('pool', lambda e, l=l: e.tensor_copy(poolbd[:, l, :], stage[:, 0, 0:256]), reads=['stg0'], writes=['poolbd'])
        c.op('dve', lambda e: e.memset(ones32[:], 1.0), writes=['ones32'])
        c.op('dve', lambda e: e.memset(vaug[:], 1.0), writes=['vaug'])
        c.op('dve', lambda e: e.memset(mbt[:], 0.0), writes=['mbt'])
        c.op('dve', lambda e: e.memset(gm[:], -1e30), writes=['gm'])
        lam_inits = [0.8 - 0.6 * math.exp(-0.3 * l) for l in range(2)]
        for l in range(2):
            dl = dlam[:, l, :].rearrange("p (a b) -> p a b", b=32)
            c.op('dve', lambda e, dl=dl: e.tensor_tensor(smallc[:, 0:32], dl[:, 0, :], dl[:, 1, :], ALU.mult), reads=['grep%d' % l], writes=['smallc'])
            c.op('dve', lambda e, l=l: e.reduce_sum(lamw[:, 8 + 2 * l:9 + 2 * l], smallc[:, 0:32], AX.X), reads=['smallc'], writes=['lamw'])
            c.op('dve', lambda e, dl=dl: e.tensor_tensor(smallc[:, 0:32], dl[:, 2, :], dl[:, 3, :], ALU.mult), reads=['grep%d' % l, 'lamw'], writes=['smallc'])
            c.op('dve', lambda e, l=l: e.reduce_sum(lamw[:, 9 + 2 * l:10 + 2 * l], smallc[:, 0:32], AX.X), reads=['smallc'], writes=['lamw'])
            c.op('act', lambda e, l=l: e.activation(lamw[:, 12 + 2 * l:14 + 2 * l], lamw[:, 8 + 2 * l:10 + 2 * l], AF.Exp), reads=['lamw'], writes=['lamw'])
            c.op('dve', lambda e, l=l: e.scalar_tensor_tensor(lamw[:, l:l + 1], lamw[:, 13 + 2 * l:14 + 2 * l], -lam_inits[l],
                                                              lamw[:, 12 + 2 * l:13 + 2 * l], ALU.add, ALU.subtract), reads=['lamw'], writes=['lamw'])
            c.op('dve', lambda e, l=l: e.tensor_scalar(lamw[:, 4 + l:5 + l], col(C_SUBG + l), 1.0 - lam_inits[l], None, ALU.mult), reads=['cols', 'lamw'], writes=['lamw'])

        def norm_group(g):
            gs = slice(g * 512, (g + 1) * 512)
            pb = 6 + nxt('nps', 2)
            for kc in range(8):
                sq = nxt('sig', 2)
                c.op('act', lambda e, kc=kc, sq=sq: e.activation(sig[:, sq, :], xT[:, kc, gs], AF.Square),
                     reads=['xT%d' % kc], writes=['sig%d' % sq])
                mm1(ps[pb][:, :], PK[pb], ones32[:], sig[:, sq, :], kc == 0, kc == 7, ['ones32', 'sig%d' % sq])
            c.op('dve', lambda e: e.tensor_scalar(bc[:, gs], ps[pb][:, :], 1.0 / D, 1e-6, ALU.mult, ALU.add),
                 reads=[], writes=[PK[pb]] + BCK)
            c.op('act', lambda e: e.sqrt(bc[:, gs], bc[:, gs]), reads=[], writes=BCK)
            c.op('dve', lambda e: e.reciprocal(bc[:, gs], bc[:, gs]), reads=[], writes=BCK)

        def norm_stats():
            for g in range(4):
                norm_group(g)

        def build_h_group(gbase, g):
            gs = slice(g * 512, (g + 1) * 512)
            for kc in range(8):
                c.op('dve', lambda e, kc=kc: e.scalar_tensor_tensor(hT[:, kc, gs], xT[:, kc, gs], col(gbase + kc), bc[:, gs], ALU.mult, ALU.mult),
                     reads=['xT%d' % kc, 'cols'] + BCK, writes=['hT%d_%d' % (kc, g)])

        def build_h(gbase):
            for kc in range(8):
                eng = 'dve'
                c.op(eng, lambda e, kc=kc: e.scalar_tensor_tensor(hT[:, kc, :], xT[:, kc, :], col(gbase + kc), bc, ALU.mult, ALU.mult),
                     reads=['xT%d' % kc, 'cols'] + BCK, writes=['hT%d_%d' % (kc, g) for g in range(4)])

        def HKg(g):
            return ['hT%d_%d' % (k, g) for k in range(8)]

        HK = ['hT%d_%d' % (k, g) for k in range(8) for g in range(4)]

        def proj_fm(l, blk, evac):
            w, wk = wload('win', (l, blk), 8)
            for g in range(4):
                pb = 6 + nxt('pps', 2)
                gs = slice(g * 512, (g + 1) * 512)
                mm_group(ps[pb][:, :], PK[pb], [(w[:, kc, :], hT[:, kc, gs]) for kc in range(8)], HKg(g) + [wk])
                evac(g, pb)

        def proj_tm(l, blk, evac4):
            w, wk = wload('win', (l, blk), 8)
            for g in range(4):
                pb = 6 + nxt('pps', 2)
                for j in range(4):
                    kt = g * 4 + j
                    mm_group(ps[pb][:, j * 128:(j + 1) * 128], PK[pb],
                             [(hT[:, kc, kt * 128:(kt + 1) * 128], w[:, kc, :]) for kc in range(8)], HKg(g) + [wk])
                evac4(g, pb)

        def attention(units, jj, vsl, bias_j, farcol, obanks, finalize, LA=2):
            steps = []
            for QG in range(4):
                nk = 4 * QG + 4
                for kt in range(nk):
                    for m in range(len(units)):
                        steps.append((QG, kt, m, nk))

            def emit_S(QG, kt, m, nk):
                kfn, qfn, rk = units[m]
                q0 = max(QG * 512, kt * 128)
                n = (QG + 1) * 512 - q0
                col0 = q0 - QG * 512
                sbk = (0, 1, 2, 7)[nxt('sps', 4)]
                mm_group(ps[sbk][:, 0:n], PK[sbk], [(kfn(kt), qfn(q0, n))], rk)
                eb = nxt('E', 4)
                ek = 'E%d' % eb
                if q0 == kt * 128:
                    nn = min(2, n // 128)
                    bsl = biasT[:, bias_j, 0:nn * 128]
                elif q0 == (kt + 1) * 128:
                    nn = 1
                    bsl = biasT[:, bias_j, 128:256]
                else:
                    nn = 0
                    bsl = None
                if nn > 0:
                    tb = nxt('tmpn', 2)
                    c.op('dve', lambda e: e.tensor_tensor(tmpn[:, tb, 0:nn * 128], ps[sbk][:, 0:nn * 128], bsl, ALU.add),
                         reads=['biasT%d' % bias_j], writes=[PK[sbk], 'tmpn%d' % tb])
                    c.op('act', lambda e: e.activation(Ebuf[:, eb, 0:nn * 128], tmpn[:, tb, 0:nn * 128], AF.Exp),
                         reads=['tmpn%d' % tb], writes=[ek])
                if n > nn * 128:
                    c.op('act', lambda e: e.activation(Ebuf[:, eb, nn * 128:n], ps[sbk][:, nn * 128:n], AF.Exp, bias=farcol, scale=1.0),
                         reads=['cols'], writes=[PK[sbk], ek])
                return (QG, kt, m, nk, eb, n, col0)

            def emit_PV(QG, kt, m, nk, eb, n, col0):
                ob = obanks[QG % 2]
                mm1(ps[ob[m]][:, col0:col0 + n], PK[ob[m]], vaug[:, kt, vsl], Ebuf[:, eb, 0:n], kt == 0, kt == nk - 1,
                    ['vaug', 'E%d' % eb])
                if kt == nk - 1 and m == len(units) - 1:
                    finalize(QG, ob)

            pend = []
            for i in range(len(steps) + LA):
                if i < len(steps):
                    pend.append(emit_S(*steps[i]))
                if i >= LA:
                    emit_PV(*pend[i - LA])

        def load_bias(h0):
            for j in range(2):
                dma_in(biasT[:, j, :], bias_d[h0 + j], 'biasT%d' % j)
                c.op('dve', lambda e, j=j: e.tensor_tensor(biasT[:, j, 0:128], biasT[:, j, 0:128], cmask[:], ALU.add),
                     reads=['cmask'], writes=['biasT%d' % j])

        def v_proj(l, blk):
            def evac4(g, pb):
                pv = ps[pb][:, :].rearrange("p (a h d) -> p a h d", a=4, h=2)
                c.op('dve', lambda e, g=g, pv=pv: e.tensor_copy(vaug[:, 4 * g:4 * g + 4, 0:64], pv[:, :, 0, :]),
                     reads=[], writes=[PK[pb], 'vaug'])
                c.op('act', lambda e, g=g, pv=pv: e.copy(vaug[:, 4 * g:4 * g + 4, 128:192], pv[:, :, 1, :]),
                     reads=[], writes=[PK[pb], 'vaug'])
            proj_tm(l, blk, evac4)

        QA, QB, KA, KB = 8, 9, 10, 11

        def moba_pair(l, cpair):
            q32 = sc32(6)
            q32k = sck(6)
            load_bias(2 * cpair)

            def evac_q(g, pb):
                gs = slice(g * 512, (g + 1) * 512)
                c.op('act', lambda e: e.mul(ct(QA)[0:64, gs], ps[pb][0:64, :], 0.125), writes=[PK[pb], ctk(QA)])
                c.op('act', lambda e: e.mul(ct(QB)[0:64, gs], ps[pb][64:128, :], 0.125), writes=[PK[pb], ctk(QB)])
                c.op('dve', lambda e: e.tensor_scalar(q32[:, gs], ps[pb][:, :], 0.125, None, ALU.mult), writes=[PK[pb]] + q32k)
            proj_fm(l, cpair, evac_q)

            def evac_k(g, pb):
                gs = slice(g * 512, (g + 1) * 512)
                c.op('act', lambda e: e.copy(ct(KA)[0:64, gs], ps[pb][0:64, :]), writes=[PK[pb], ctk(KA)])
                c.op('act', lambda e: e.copy(ct(KB)[0:64, gs], ps[pb][64:128, :]), writes=[PK[pb], ctk(KB)])
                c.op('dve', lambda e: e.tensor_reduce(ksum[:, 2 * g:2 * g + 2], ps[pb][:, :].rearrange("p (a b) -> p a b", b=256), AX.X, ALU.add),
                     writes=[PK[pb], 'ksum'])
            proj_fm(l, 2 + cpair, evac_k)
            for t in (KA, KB):
                dma_in(ct(t)[64:72, :], kblk_d, ctk(t), sem='kb%d' % t)
            for t in (QA, QB):
                c.op('dve', lambda e, t=t: e.memset(ct(t)[64:72, 0:1024], 0.0), writes=[ctk(t)])
            c.op('dve', lambda e: e.tensor_scalar(ksum[:, :], ksum[:, :], 1.0 / 256, None, ALU.mult), writes=['ksum'])
            for j in range(2):
                hs = slice(64 * j, 64 * j + 64)
                for qt in range(8, 16):
                    mm_group(ps[5][:, j * 128 + qt * 8:j * 128 + qt * 8 + 8], PK[5],
                             [(q32[hs, qt * 128:(qt + 1) * 128], ksum[hs, 0:8])], q32k + ['ksum'])
            c.op('dve', lambda e: e.tensor_copy(gsb[:, :, 0:8, :], ps[5][:, 0:256].rearrange("p (j q b) -> p j q b", j=2, q=16)[:, :, 8:16, :]),
                 writes=[PK[5], 'gsb'])
            v_proj(l, 4 + cpair)
            for own in range(4, 8):
                c.op('dve', lambda e, own=own: e.tensor_copy(gm[:, :, 2 * own - 8:2 * own - 6, 0:own], gsb[:, :, 2 * own - 8:2 * own - 6, 0:own]),
                     reads=['gsb'], writes=['gm'])
            for j in range(2):
                qt_tile = QA if j == 0 else QB
                for qt in range(8, 16):
                    own = qt // 2
                    c.op('dve', lambda e, j=j, qt=qt: e.max(m8[:, qt, :], gm[:, j, qt - 8, :]), reads=['gm'], writes=['m8_%d' % qt])
                    c.op('dve', lambda e, j=j, qt=qt, own=own: e.tensor_scalar(mbt[:, j, qt - 8, 0:own], gm[:, j, qt - 8, 0:own], m8[:, qt, 2:3], NEG,
                                                                             ALU.is_lt, ALU.mult),
                         reads=['gm', 'm8_%d' % qt], writes=['mbt'])
                for half in range(2):
                    for i in range(4):
                        qt = 8 + half * 4 + i
                        c.op('pe', lambda e, j=j, qt=qt, i=i: e.transpose(ps[5][0:8, i * 128:(i + 1) * 128], mbt[:, j, qt - 8, :], ident[:]),
                             reads=['mbt', 'ident'], writes=[PK[5]])
                    c.op('act', lambda e, half=half, qt_tile=qt_tile: e.copy(
                        ct(qt_tile)[64:72, 1024 + half * 512:1536 + half * 512], ps[5][0:8, :]),
                        writes=[PK[5], ctk(qt_tile)])
            for j in range(2):
                qt_tile, kt_tile = (QA, KA) if j == 0 else (QB, KB)
                units = [(lambda kt, kt_tile=kt_tile: ct(kt_tile)[0:72, kt * 128:(kt + 1) * 128],
                          lambda q0, n, qt_tile=qt_tile: ct(qt_tile)[0:72, q0:q0 + n],
                          [ctk(qt_tile), ctk(kt_tile)])]
                vsl = slice(0, 128) if j == 0 else slice(64, 192)
                num = slice(64 * j, 64 * j + 64)
                den = slice(64 * (1 - j), 64 * (1 - j) + 64)

                def fin(QG, ob, num=num, den=den):
                    gs = slice(QG * 512, (QG + 1) * 512)
                    rb = nxt('rden', 2)
                    c.op('dve', lambda e: e.reciprocal(rden[num, rb, :], ps[ob[0]][den, :]), writes=[PK[ob[0]], 'rden%d' % rb])
                    c.op('dve', lambda e: e.tensor_tensor(ct(cpair)[num, gs], ps[ob[0]][num, :], rden[num, rb, :], ALU.mult),
                         reads=['rden%d' % rb], writes=[PK[ob[0]], ctk(cpair)])
                attention(units, j, vsl, j, col(C_FAR + 2 * cpair + j), [(3,), (4,)], fin)

        def diff_pair(l, cpair):
            osc = sc32(7)
            osck = sck(7)
            load_bias(4 + 2 * cpair)
            qs = 32 ** -0.5

            def evac_q(g, pb):
                gs = slice(g * 512, (g + 1) * 512)
                c.op('act', lambda e: e.mul(ct(QA)[0:64, gs], ps[pb][0:64, :], qs), writes=[PK[pb], ctk(QA)])
                c.op('dve', lambda e: e.tensor_scalar(ct(QB)[0:64, gs], ps[pb][64:128, :], qs, None, ALU.mult), writes=[PK[pb], ctk(QB)])
            proj_fm(l, 14 + cpair, evac_q)

            KT = {(0, 0): 10, (0, 1): 11, (1, 0): 12, (1, 1): 13}

            def evac_k(g, pb):
                gs = slice(g * 512, (g + 1) * 512)
                for j in range(2):
                    src = ps[pb][64 * j:64 * j + 64, :]
                    for m in range(2):
                        t = KT[(j, m)]
                        if m == 0:
                            c.op('act', lambda e, t=t, src=src: e.copy(ct(t)[0:64, gs], src), writes=[PK[pb], ctk(t)])
                        else:
                            c.op('dve', lambda e, t=t, src=src: e.tensor_copy(ct(t)[0:64, gs], src), writes=[PK[pb], ctk(t)])
                        z = slice(32, 64) if m == 0 else slice(0, 32)
                        c.op('dve', lambda e, t=t, z=z: e.memset(ct(t)[z, gs], 0.0), writes=[ctk(t)])
            proj_fm(l, 16 + cpair, evac_k)
            v_proj(l, 18 + cpair)
            for j in range(2):
                qt_tile = QA if j == 0 else QB
                units = []
                for m in range(2):
                    kt_tile = KT[(j, m)]
                    units.append((lambda kt, kt_tile=kt_tile: ct(kt_tile)[0:64, kt * 128:(kt + 1) * 128],
                                  lambda q0, n, qt_tile=qt_tile: ct(qt_tile)[0:64, q0:q0 + n],
                                  [ctk(qt_tile), ctk(kt_tile)]))
                vsl = slice(0, 128) if j == 0 else slice(64, 192)
                num = slice(64 * j, 64 * j + 64)
                den = slice(64 * (1 - j), 64 * (1 - j) + 64)

                def fin(QG, ob, num=num, den=den):
                    gs = slice(QG * 512, (QG + 1) * 512)
                    c.op('dve', lambda e: e.reciprocal(rden[num, 0, :], ps[ob[0]][den, :]), writes=[PK[ob[0]], 'rden0'])
                    c.op('dve', lambda e: e.tensor_tensor(osc[num, gs], ps[ob[0]][num, :], rden[num, 0, :], ALU.mult),
                         reads=['rden0'], writes=[PK[ob[0]]] + osck)
                    c.op('dve', lambda e: e.reciprocal(rden[num, 1, :], ps[ob[1]][den, :]), writes=[PK[ob[1]], 'rden1'])
                    c.op('dve', lambda e: e.tensor_tensor(rden[num, 1, :], ps[ob[1]][num, :], rden[num, 1, :], ALU.mult),
                         writes=[PK[ob[1]], 'rden1'])
                    c.op('dve', lambda e: e.scalar_tensor_tensor(osc[num, gs], rden[num, 1, :], lamw[num, l:l + 1], osc[num, gs], ALU.mult, ALU.add),
                         reads=['rden1', 'lamw'], writes=osck)
                attention(units, j, vsl, j, col(C_FAR + 4 + 2 * cpair + j), [(3, 4), (5, 6)], fin)
            for g in range(4):
                gs = slice(g * 512, (g + 1) * 512)
                sq = nxt('sig', 2)
                c.op('act', lambda e, sq=sq, gs=gs: e.activation(sig[:, sq, :], osc[:, gs], AF.Square), reads=osck, writes=['sig%d' % sq])
                pb = 6 + nxt('pps', 2)
                mm1(ps[pb][:, :], PK[pb], bones[:], sig[:, sq, :], True, True, ['bones', 'sig%d' % sq])
                c.op('dve', lambda e, pb=pb: e.tensor_scalar(rden[:, 0, :], ps[pb][:, :], 1.0 / 64, 1e-5, ALU.mult, ALU.add), writes=[PK[pb], 'rden0'])
                c.op('act', lambda e: e.sqrt(rden[:, 0, :], rden[:, 0, :]), writes=['rden0'])
                c.op('dve', lambda e: e.reciprocal(rden[:, 0, :], rden[:, 0, :]), writes=['rden0'])
                c.op('dve', lambda e, gs=gs: e.scalar_tensor_tensor(ct(6 + cpair)[:, gs], osc[:, gs], lamw[:, 4 + l:5 + l], rden[:, 0, :], ALU.mult, ALU.mult),
                     reads=osck + ['lamw', 'rden0'], writes=[ctk(6 + cpair)])

        def conv_chunk(l, cc):
            XB, BB, CB = 8, 9, 10
            u = sc32(6)
            uk = sck(6)
            acc = sc32(7)
            acck = sck(7)

            def mk_evac(tile):
                def ev(g, pb):
                    gs = slice(g * 512, (g + 1) * 512)
                    eng = 'act' if g % 2 == 0 else 'dve'
                    if eng == 'act':
                        c.op('act', lambda e: e.copy(ct(tile)[:, gs], ps[pb][:, :]), writes=[PK[pb], ctk(tile)])
                    else:
                        c.op('dve', lambda e: e.tensor_copy(ct(tile)[:, gs], ps[pb][:, :]), writes=[PK[pb], ctk(tile)])
                return ev
            proj_fm(l, 6 + cc, mk_evac(XB))
            proj_fm(l, 8 + cc, mk_evac(BB))
            proj_fm(l, 10 + cc, mk_evac(CB))
            w0 = col(C_CONV + (l * 2 + cc) * 3 + 0)
            w1 = col(C_CONV + (l * 2 + cc) * 3 + 1)
            w2 = col(C_CONV + (l * 2 + cc) * 3 + 2)
            c.op('dve', lambda e: e.tensor_tensor(u, ct(CB), ct(XB), ALU.mult), reads=[ctk(CB), ctk(XB)], writes=uk)
            c.op('dve', lambda e: e.tensor_scalar(acc, u, w2, None, ALU.mult), reads=uk + ['cols'], writes=acck)
            c.op('dve', lambda e: e.scalar_tensor_tensor(acc[:, 1:S], u[:, 0:S - 1], w1, acc[:, 1:S], ALU.mult, ALU.add), reads=uk + ['cols'], writes=acck)
            c.op('dve', lambda e: e.scalar_tensor_tensor(acc[:, 2:S], u[:, 0:S - 2], w0, acc[:, 2:S], ALU.mult, ALU.add), reads=uk + ['cols'], writes=acck)
            c.op('dve', lambda e: e.tensor_tensor(ct(2 + cc), acc, ct(BB), ALU.mult), reads=acck + [ctk(BB)], writes=[ctk(2 + cc)])

        def pool_chunk(l, cc):
            u = sc32(4)
            uk = sck(4)
            s1 = sc32(5)
            s1k = sck(5)
            s2 = sc32(6)
            s2k = sck(6)
            PT = 14

            def ev(g, pb):
                gs = slice(g * 512, (g + 1) * 512)
                c.op('act', lambda e: e.copy(u[:, gs], ps[pb][:, :]), writes=[PK[pb]] + uk)
            proj_fm(l, 12 + cc, ev)
            hi = slice(64, 128)
            c.op('dve', lambda e: e.tensor_copy(s1[:, 0:1], u[:, 0:1]), reads=uk, writes=s1k)
            c.op('dve', lambda e: e.tensor_tensor(s1[:, 1:S], u[:, 1:S], u[:, 0:S - 1], ALU.add), reads=uk, writes=s1k)
            if cc == 0:
                c.op('dve', lambda e: e.tensor_copy(s2[hi, 0:2], s1[hi, 0:2]), reads=s1k, writes=s2k)
                c.op('dve', lambda e: e.tensor_tensor(s2[hi, 2:S], s1[hi, 2:S], s1[hi, 0:S - 2], ALU.add), reads=s1k, writes=s2k)
                c.op('dve', lambda e: e.tensor_copy(s2[0:64, :], s1[0:64, :]), reads=s1k, writes=s2k)
                fin = s2
                fink = s2k
                wlo, whi = 2, 4
            else:
                c.op('dve', lambda e: e.tensor_copy(s2[:, 0:2], s1[:, 0:2]), reads=s1k, writes=s2k)
                c.op('dve', lambda e: e.tensor_tensor(s2[:, 2:S], s1[:, 2:S], s1[:, 0:S - 2], ALU.add), reads=s1k, writes=s2k)
                c.op('dve', lambda e: e.tensor_copy(s1[:, 0:4], s2[:, 0:4]), reads=s2k, writes=s1k)
                c.op('dve', lambda e: e.tensor_tensor(s1[:, 4:S], s2[:, 4:S], s2[:, 0:S - 4], ALU.add), reads=s2k, writes=s1k)
                c.op('dve', lambda e: e.tensor_copy(s2[hi, 0:8], s1[hi, 0:8]), reads=s1k, writes=s2k)
                c.op('dve', lambda e: e.tensor_tensor(s2[hi, 8:S], s1[hi, 8:S], s1[hi, 0:S - 8], ALU.add), reads=s1k, writes=s2k)
                c.op('dve', lambda e: e.tensor_copy(s2[0:64, :], s1[0:64, :]), reads=s1k, writes=s2k)
                fin = s2
                fink = s2k
                wlo, whi = 8, 16
            lo = slice(0, 64)
            c.op('dve', lambda e: e.scalar_tensor_tensor(ct(PT)[lo, :], fin[lo, :], 1.0 / wlo, u[lo, :], ALU.mult, ALU.subtract), reads=fink + uk, writes=[ctk(PT)])
            c.op('dve', lambda e: e.scalar_tensor_tensor(ct(PT)[hi, :], fin[hi, :], 1.0 / whi, u[hi, :], ALU.mult, ALU.subtract), reads=fink + uk, writes=[ctk(PT)])
            c.op('dve', lambda e: e.tensor_tensor(fin[:, 0:16], fin[:, 0:16], rc[:, 16 * cc:16 * cc + 16], ALU.mult), reads=['rc'], writes=fink)
            c.op('dve', lambda e: e.tensor_tensor(ct(PT)[:, 0:16], fin[:, 0:16], u[:, 0:16], ALU.subtract), reads=fink + uk, writes=[ctk(PT)])
            for g in range(4):
                gs = slice(g * 512, (g + 1) * 512)
                pb = 6 + nxt('pps', 2)
                mm_group(ps[pb][:, :], PK[pb], [(poolbd[:, l, 128 * cc:128 * cc + 128], ct(PT)[:, gs])], ['poolbd', ctk(PT)])
                c.op('act', lambda e, pb=pb, gs=gs: e.mul(ct(4 + cc)[:, gs], ps[pb][:, :], col(C_PSC + 2 * l + cc)),
                     reads=['cols'], writes=[PK[pb], ctk(4 + cc)])

        def merge_out(l, ffn_gbase):
            MG = 8
            mview = arena[:, MG * S:(MG + 2) * S].rearrange("p (a b) -> p a b", b=512)
            mk = [ctk(MG), ctk(MG + 1)]
            macc = rden
            for g in range(4):
                gs = slice(g * 512, (g + 1) * 512)
                for dc in range(8):
                    for br in range(4):
                        wg, wgk = wload('win', (l, 20 + br * 8 + dc), 8)
                        wb, wbk = wload('bp', (l, br, dc), 2)
                        pg = nxt('mps', 2) * 2
                        mm_group(ps[pg][:, :], PK[pg], [(wg[:, kc, :], hT[:, kc, gs]) for kc in range(8)], HKg(g) + [wgk])
                        mm_group(ps[pg + 1][:, :], PK[pg + 1], [(wb[:, kc, :], ct(2 * br + kc)[:, gs]) for kc in range(2)],
                                 [wbk, ctk(2 * br), ctk(2 * br + 1)])
                        sq = nxt('sig', 2)
                        c.op('act', lambda e, pg=pg, sq=sq: e.activation(sig[:, sq, :], ps[pg][:, :], AF.Sigmoid), writes=[PK[pg], 'sig%d' % sq])
                        if br == 0:
                            c.op('dve', lambda e, pg=pg, sq=sq: e.tensor_tensor(macc[:, 0, :], sig[:, sq, :], ps[pg + 1][:, :], ALU.mult),
                                 reads=['sig%d' % sq], writes=[PK[pg + 1], 'rden0'])
                        else:
                            c.op('dve', lambda e, pg=pg, sq=sq: e.tensor_tensor(macc[:, 1, :], sig[:, sq, :], ps[pg + 1][:, :], ALU.mult),
                                 reads=['sig%d' % sq], writes=[PK[pg + 1], 'rden1'])
                            if br < 3:
                                c.op('dve', lambda e: e.tensor_tensor(macc[:, 0, :], macc[:, 0, :], macc[:, 1, :], ALU.add),
                                     reads=['rden1'], writes=['rden0'])
                            else:
                                c.op('dve', lambda e, dc=dc: e.tensor_tensor(mview[:, dc, :], macc[:, 0, :], macc[:, 1, :], ALU.add),
                                     reads=['rden1', 'rden0'], writes=mk)
                for do in range(8):
                    w, wk = wload('wout', (l, do), 8)
                    pb = 4 + nxt('ops', 2)
                    mm_group(ps[pb][:, :], PK[pb], [(w[:, kc, :], mview[:, kc, :]) for kc in range(8)], mk + [wk])
                    c.op('dve', lambda e, pb=pb, do=do, gs=gs: e.tensor_tensor(xT[:, do, gs], xT[:, do, gs], ps[pb][:, :], ALU.add),
                         writes=[PK[pb], 'xT%d' % do])
                norm_group(g)
                build_h_group(ffn_gbase, g)

        def ffn(experts):
            work = []
            for ei, (w1f, w3f, w2f, nf, ge) in enumerate(experts):
                for pi, (f0, fp) in enumerate(split_parts(nf, 7)):
                    work.append((ei, pi, f0, fp))

            def phaseA(wi):
                ei, pi, f0, fp = work[wi]
                w1f, w3f, w2f, nf, ge = experts[ei]
                aset = (wi % 2) * 7
                if ge is not None and pi == 0:
                    build_gbc(ge)
                for j in range(fp):
                    fi = f0 + j
                    w1, w1k = wload(*w1f(fi))
                    w3, w3k = wload(*w3f(fi))
                    at = aset + j
                    for g in range(4):
                        gs = slice(g * 512, (g + 1) * 512)
                        pb = nxt('fps', 2) * 2
                        mm_group(ps[pb][:, :], PK[pb], [(w1[:, kc, :], hT[:, kc, gs]) for kc in range(8)], HKg(g) + [w1k])
                        mm_group(ps[pb + 1][:, :], PK[pb + 1], [(w3[:, kc, :], hT[:, kc, gs]) for kc in range(8)], HKg(g) + [w3k])
                        sq = nxt('sig', 2)
                        c.op('act', lambda e, pb=pb, sq=sq: e.activation(sig[:, sq, :], ps[pb][:, :], AF.Silu), writes=[PK[pb], 'sig%d' % sq])
                        if ge is None:
                            c.op('dve', lambda e, pb=pb, sq=sq, at=at, gs=gs: e.tensor_tensor(ct(at)[:, gs], sig[:, sq, :], ps[pb + 1][:, :], ALU.mult),
                                 reads=['sig%d' % sq], writes=[PK[pb + 1], ctk(at)])
                        else:
                            c.op('dve', lambda e, pb=pb, sq=sq, gs=gs: e.tensor_tensor(sig[:, sq, :], sig[:, sq, :], ps[pb + 1][:, :], ALU.mult),
                                 writes=[PK[pb + 1], 'sig%d' % sq])
                            c.op('dve', lambda e, sq=sq, at=at, gs=gs: e.tensor_tensor(ct(at)[:, gs], sig[:, sq, :], bc[:, gs], ALU.mult),
                                 reads=['sig%d' % sq] + BCK, writes=[ctk(at)])

            def phaseB(wi):
                ei, pi, f0, fp = work[wi]
                w1f, w3f, w2f, nf, ge = experts[ei]
                aset = (wi % 2) * 7
                for dc in range(8):
                    w2, w2k = wload(*w2f(dc, f0, fp))
                    for g in range(4):
                        gs = slice(g * 512, (g + 1) * 512)
                        pb = 4 + nxt('ops', 2)
                        mm_group(ps[pb][:, :], PK[pb], [(w2[:, j, :], ct(aset + j)[:, gs]) for j in range(fp)],
                                 [w2k] + [ctk(aset + j) for j in range(fp)])
                        c.op('dve', lambda e, pb=pb, dc=dc, gs=gs: e.tensor_tensor(xT[:, dc, gs], xT[:, dc, gs], ps[pb][:, :], ALU.add),
                             writes=[PK[pb], 'xT%d' % dc])
            phaseA(0)
            for wi in range(len(work)):
                if wi + 1 < len(work):
                    phaseA(wi + 1)
                phaseB(wi)

        def build_gbc(ge):
            for g in range(4):
                pb = 6 + nxt('pps', 2)
                for j in range(4):
                    kt = 4 * g + j
                    gb = nxt('grep', 2)
                    c.op('dve', lambda e, gb=gb, kt=kt: e.tensor_copy(grep[:, gb, :], gtok[:, kt, ge:ge + 1].to_broadcast([128, 128])),
                         reads=['gtok'], writes=['grep%d' % gb])
                    mm_group(ps[pb][:, j * 128:(j + 1) * 128], PK[pb], [(grep[:, gb, :], ident[:])], ['grep%d' % gb, 'ident'])
                c.op('act', lambda e, pb=pb, g=g: e.copy(bc[:, g * 512:(g + 1) * 512], ps[pb][:, :]), writes=[PK[pb]] + BCK)

        def router(gbase):
            for g in range(4):
                gs = slice(g * 512, (g + 1) * 512)
                for kc in range(8):
                    sq = nxt('sig', 2)
                    c.op('dve', lambda e, kc=kc, sq=sq, gs=gs: e.scalar_tensor_tensor(sig[:, sq, :], xT[:, kc, gs], col(gbase + kc), bc[:, gs], ALU.mult, ALU.mult),
                         reads=['xT%d' % kc, 'cols'] + BCK, writes=['sig%d' % sq])
                    mm1(ps[6][0:8, :], PK[6], router32[:, kc * 8:kc * 8 + 8], sig[:, sq, :], kc == 0, kc == 7, ['router32', 'sig%d' % sq])
                c.op('act', lambda e: e.copy(rden[0:8, 0, :], ps[6][0:8, :]), writes=[PK[6], 'rden0'])
                for j in range(4):
                    c.op('pe', lambda e, j=j: e.transpose(ps[7][:, j * 8:j * 8 + 8], rden[0:8, 0, j * 128:(j + 1) * 128], ident[0:8, 0:8]),
                         reads=['rden0', 'ident'], writes=[PK[7]])
                c.op('dve', lambda e, g=g: e.tensor_copy(lg[:, 4 * g:4 * g + 4, :], ps[7][:, 0:32].rearrange("p (a b) -> p a b", b=8)),
                     writes=[PK[7], 'lg'])
            for kt in range(16):
                mk_ = 'm8_%d' % kt
                c.op('dve', lambda e, kt=kt: e.max(m8[:, kt, :], lg[:, kt, :]), reads=['lg'], writes=[mk_])
                c.op('dve', lambda e, kt=kt: e.tensor_scalar(smallc[:, kt:kt + 1], m8[:, kt, 0:1], -1.0, None, ALU.mult), reads=[mk_], writes=['smallc'])
                c.op('act', lambda e, kt=kt: e.activation(ex[:, kt, :], lg[:, kt, :], AF.Exp, bias=smallc[:, kt:kt + 1], scale=1.0),
                     reads=['lg', 'smallc'], writes=['ex'])
                c.op('dve', lambda e, kt=kt: e.tensor_scalar(gtok[:, kt, :], lg[:, kt, :], m8[:, kt, 1:2], None, ALU.is_ge), reads=['lg', mk_], writes=['gtok'])
                c.op('dve', lambda e, kt=kt: e.tensor_tensor(ex[:, kt, :], ex[:, kt, :], gtok[:, kt, :], ALU.mult), reads=['gtok'], writes=['ex'])
                c.op('dve', lambda e, kt=kt: e.reduce_sum(smallc[:, 16 + kt:17 + kt], ex[:, kt, :], AX.X), reads=['ex'], writes=['smallc'])
                c.op('dve', lambda e, kt=kt: e.reciprocal(smallc[:, 32 + kt:33 + kt], smallc[:, 16 + kt:17 + kt]), writes=['smallc'])
                c.op('dve', lambda e, kt=kt: e.tensor_scalar(gtok[:, kt, :], ex[:, kt, :], smallc[:, 32 + kt:33 + kt], None, ALU.mult),
                     reads=['ex', 'smallc'], writes=['gtok'])

        XK = ['xT%d' % k for k in range(8)]

        def dump_x(s):
            toks = []
            for kc in range(8):
                toks.append(c.op('sp', lambda e, kc=kc: e.dma_start(out=out_d[s, kc], in_=xT[:, kc, :]), reads=['xT%d' % kc], dma='sx%d' % kc))
            return toks

        def dump_tiles(s, tiles):
            toks = []
            for i, t in enumerate(tiles):
                k = i % 2
                c.op('dve', lambda e, t=t, k=k: e.tensor_copy(sc32(6 + k), ct(t)), reads=[ctk(t)], writes=sck(6 + k))
                toks.append(c.op('sp', lambda e, i=i, k=k: e.dma_start(out=out_d[s, i], in_=sc32(6 + k)), reads=sck(6 + k), dma='sd%d' % k))
            return toks

        out_toks = []
        for s in range(nseq):
            for kc in range(8):
                dma_in(xT[:, kc, :], xT_d[s, kc], 'xT%d' % kc, sem='xld%d' % kc)
            done = False
            for l in range(2):
                norm_stats()
                build_h(C_MIXG + 8 * l)
                if stop == 'h%d' % l:
                    c.op('dve', lambda e: e.tensor_copy(ct(0), hT[:, 0, :]), reads=HK, writes=[ctk(0)])
                    c.op('dve', lambda e: e.tensor_copy(ct(1), hT[:, 7, :]), reads=HK, writes=[ctk(1)])
                    out_toks += dump_tiles(s, [0, 1])
                    done = True
                    break
                for cp in range(2):
                    moba_pair(l, cp)
                if stop == 'a%d' % l:
                    out_toks += dump_tiles(s, [0, 1])
                    done = True
                    break
                for cc in range(2):
                    conv_chunk(l, cc)
                for cc in range(2):
                    pool_chunk(l, cc)
                for cp in range(2):
                    diff_pair(l, cp)
                if stop == 'y%d' % l:
                    out_toks += dump_tiles(s, list(range(8)))
                    done = True
                    break
                merge_out(l, C_FFNG + 8 * l)
                if stop == 'mix%d' % l:
                    out_toks += dump_x(s)
                    done = True
                    break
                if l == 0:
                    ffn([(lambda fi: ('dw1', (fi,), 8), lambda fi: ('dw3', (fi,), 8),
                          lambda dc, f0, fp: ('dw2', (dc,), fp, slice(f0 * 128, (f0 + fp) * 128)), 22, None)])
                else:
                    router(C_FFNG + 8 * l)
                    ffn([(lambda fi, e_=e_: ('mw1', (e_, fi), 8), lambda fi, e_=e_: ('mw3', (e_, fi), 8),
                          lambda dc, f0, fp, e_=e_: ('mw2', (e_, dc), fp, slice(f0 * 128, (f0 + fp) * 128)), 28, e_) for e_ in range(8)])
                if stop == 'ffn%d' % l:
                    out_toks += dump_x(s)
                    done = True
                    break
            if done:
                continue
            norm_stats()
            for kc in range(8):
                k = kc % 4
                eng = 'dve'
                c.op(eng, lambda e, kc=kc, k=k: e.scalar_tensor_tensor(sc32(k), xT[:, kc, :], col(C_FING + kc), bc, ALU.mult, ALU.mult),
                     reads=['xT%d' % kc, 'cols'] + BCK, writes=sck(k))
                out_toks.append(c.op('sp', lambda e, kc=kc, k=k, s=s: e.dma_start(out=out_d[s, kc], in_=sc32(k)), reads=sck(k), dma='st%d' % k))
        flush_store()
        c.final_wait('sp', out_toks)
        c.emit()
    return nc


def _rel_bucket_np(dist):
    n = np.maximum(dist, 0)
    max_exact = 16
    nf = np.maximum(n, max_exact).astype(np.float32)
    large = max_exact + (np.log(nf / np.float32(max_exact)) / np.float32(math.log(128 / max_exact)) * np.float32(16)).astype(np.int32)
    large = np.minimum(large, 31)
    return np.where(n < max_exact, n, large)


def wblocks(W, nb=None):
    K, N = W.shape
    kc = K // 128
    nbk = N // 128
    a = W.reshape(kc, 128, nbk, 128).transpose(2, 1, 0, 3)
    return np.ascontiguousarray(a).reshape(nbk, 128, kc * 128)


def prep_shared(inp):
    f = np.float32
    sh = {}
    sh['w_in'] = np.stack([wblocks(np.asarray(inp['w_in'][l], f)) for l in range(2)])
    bp = np.asarray(inp['branch_proj'], f)
    sh['bp'] = np.stack([np.stack([wblocks(bp[l, b]) for b in range(4)]) for l in range(2)])
    sh['w_out'] = np.stack([wblocks(np.asarray(inp['w_out'][l], f)) for l in range(2)])
    sh['dw1'] = wblocks(np.asarray(inp['dense_w1'][0], f))
    sh['dw3'] = wblocks(np.asarray(inp['dense_w3'][0], f))
    sh['dw2'] = wblocks(np.asarray(inp['dense_w2'][0], f))
    sh['mw1'] = np.stack([wblocks(np.asarray(inp['moe_w1'][0, e], f)) for e in range(8)])
    sh['mw3'] = np.stack([wblocks(np.asarray(inp['moe_w3'][0, e], f)) for e in range(8)])
    sh['mw2'] = np.stack([wblocks(np.asarray(inp['moe_w2'][0, e], f)) for e in range(8)])
    r = np.asarray(inp['moe_router'][0], f)
    sh['router'] = np.ascontiguousarray(r.reshape(8, 128, 8).transpose(1, 0, 2)).reshape(128, 64)
    cols = np.zeros((128, NCOL), f)

    def chunkcols(v):
        return np.asarray(v, f).reshape(-1, 128).T
    for l in range(2):
        cols[:, C_MIXG + 8 * l:C_MIXG + 8 * l + 8] = chunkcols(inp['mix_norm_g'][l])
        cols[:, C_FFNG + 8 * l:C_FFNG + 8 * l + 8] = chunkcols(inp['ffn_norm_g'][l])
        cw = np.asarray(inp['conv_w'][l], f)
        for cc in range(2):
            for i in range(3):
                cols[:, C_CONV + (l * 2 + cc) * 3 + i] = cw[i, cc * 128:(cc + 1) * 128]
        cols[:, C_PSC + 2 * l:C_PSC + 2 * l + 2] = chunkcols(inp['pool_scale'][l])
        sg = np.asarray(inp['diff_subln_g'][l], f)
        cols[:, C_SUBG + l] = np.concatenate([sg, sg])
    cols[:, C_FING:C_FING + 8] = chunkcols(inp['final_norm_g'])
    bt = np.asarray(inp['bias_table'], f)
    for h in range(8):
        cols[:, C_FAR + h] = bt[31, h]
    sh['cols'] = cols
    s_idx = np.arange(128)[:, None]
    t_idx = np.arange(128)[None, :]
    bd = _rel_bucket_np(t_idx - s_idx)
    bs = _rel_bucket_np(128 + t_idx - s_idx)
    biasT = np.zeros((8, 128, 256), f)
    for h in range(8):
        biasT[h, :, 0:128] = bt[bd, h]
        biasT[h, :, 128:256] = bt[bs, h]
    sh['biasT'] = biasT
    sh['cmask'] = np.where(t_idx >= s_idx, 0.0, -1e30).astype(f)
    sh['ident'] = np.eye(128, dtype=f)
    bones = np.zeros((128, 128), f)
    bones[0:64, 0:64] = 1.0
    bones[64:128, 64:128] = 1.0
    sh['bones'] = bones
    rc = np.zeros((128, 32), f)
    wins = (2, 4, 8, 16)
    for cc in range(2):
        for half in range(2):
            w = wins[2 * cc + half]
            rc[64 * half:64 * half + 64, 16 * cc:16 * cc + 16] = 1.0 / np.minimum(np.arange(1, 17), w).astype(f)
    sh['rc'] = rc
    pw = np.asarray(inp['pool_w'], f)
    pbd = np.zeros((2, 128, 256), f)
    for l in range(2):
        for cc in range(2):
            for half in range(2):
                pbd[l, 64 * half:64 * half + 64, 128 * cc + 64 * half:128 * cc + 64 * half + 64] = pw[l, 2 * cc + half]
    sh['poolbd'] = pbd
    dl = np.asarray(inp['diff_lambda'], f).reshape(2, 1, 128)
    sh['dlam'] = np.ascontiguousarray(np.broadcast_to(dl, (2, 128, 128)))
    kb = np.zeros((8, S), f)
    for b in range(8):
        kb[b, 256 * b:256 * (b + 1)] = 1.0
    sh['kblk'] = kb.astype(ml_dtypes.bfloat16)
    return sh


_CACHE = {}


def kernel(**inputs):
    x = np.asarray(inputs['x'], np.float32)
    sh = prep_shared(inputs)
    if 'nc' not in _CACHE:
        _CACHE['nc'] = build(NSEQ)
    nc = _CACHE['nc']
    in_maps = []
    for core in range(NCORES):
        xs = x[core * NSEQ:(core + 1) * NSEQ]
        xTt = np.ascontiguousarray(xs.transpose(0, 2, 1)).reshape(NSEQ, 8, 128, S)
        m = dict(sh)
        m['xT'] = xTt
        in_maps.append(m)
    res = run_bass_kernel_spmd(nc, in_maps, core_ids=list(range(NCORES)))
    outs = []
    for core in range(NCORES):
        o = np.asarray(res.results[core]['outT']).reshape(NSEQ, 1024, S)
        outs.append(o.transpose(0, 2, 1))
    return np.ascontiguousarray(np.concatenate(outs, axis=0)).astype(np.float32)
```

```python
import math
from contextlib import ExitStack

import numpy as np
import ml_dtypes

import concourse.bass as bass
import concourse.mybir as mybir
from concourse.bass_utils import run_bass_kernel_spmd

F32 = mybir.dt.float32
BF16 = mybir.dt.bfloat16
ALU = mybir.AluOpType
AF = mybir.ActivationFunctionType
AX = mybir.AxisListType

D = 1024
S = 2048
NCORES = 8
NSEQ = 4
NEG = -30000.0
DFF_D = 2816
DFF_E = 3584
NS_STAGE = 2
NW_RING = 4

C_MIXG = 0
C_FFNG = 16
C_FING = 32
C_CONV = 40
C_PSC = 52
C_SUBG = 56
C_FAR = 58
C_MSK = 66
NCOL = 70


class Ctx:
    ENG = ['pe', 'act', 'dve', 'pool', 'sp']

    def __init__(self, nc, es):
        self.nc = nc
        self.es = es
        self.sem = {e: es.enter_context(nc.semaphore('s_' + e)) for e in self.ENG}
        self.cnt = {e: 0 for e in self.ENG}
        self.ops = {e: [] for e in self.ENG}
        self.waited = {}
        self.lastw = {}
        self.readers = {}
        self.dma_sems = {}

    def dsem(self, name):
        if name not in self.dma_sems:
            self.dma_sems[name] = [self.es.enter_context(self.nc.semaphore('d_' + name)), 0]
        return self.dma_sems[name]

    def _need(self, eng, waits, dep):
        if dep is None:
            return
        sem, val, src = dep
        if src == 'pe' and eng == 'pe':
            return
        k = (eng, id(sem))
        if self.waited.get(k, 0) >= val:
            return
        self.waited[k] = val
        waits.append((sem, val))

    def op(self, eng, fn, reads=(), writes=(), dma=None):
        waits = []
        for key in reads:
            self._need(eng, waits, self.lastw.get(key))
        for key in writes:
            self._need(eng, waits, self.lastw.get(key))
            for d in self.readers.get(key, {}).values():
                self._need(eng, waits, d)
        if dma is None:
            self.cnt[eng] += 1
            tok = (self.sem[eng], self.cnt[eng], eng)
            inc = (self.sem[eng], 1)
        else:
            ds = self.dsem(dma)
            ds[1] += 16
            tok = (ds[0], ds[1], 'dma:' + dma)
            inc = (ds[0], 16)
        for key in writes:
            self.lastw[key] = tok
            self.readers[key] = {}
        for key in reads:
            if key not in writes:
                self.readers.setdefault(key, {})[id(tok[0])] = tok

        def run(e, waits=waits, fn=fn, inc=inc):
            for (s, v) in waits:
                e.wait_ge(s, v)
            fn(e).then_inc(inc[0], inc[1])
        self.ops[eng].append(run)
        return tok

    def final_wait(self, eng, toks):
        waits = []
        best = {}
        for t in toks:
            if id(t[0]) not in best or best[id(t[0])][1] < t[1]:
                best[id(t[0])] = t
        for t in best.values():
            self._need(eng, waits, t)

        def run(e, waits=waits):
            for (s, v) in waits:
                e.wait_ge(s, v)
        self.ops[eng].append(run)

    def emit(self):
        nc = self.nc
        ops = self.ops
        with nc.Block() as blk:
            @blk.tensor
            def _(e):
                for r in ops['pe']:
                    r(e)

            @blk.scalar
            def _(e):
                for r in ops['act']:
                    r(e)

            @blk.vector
            def _(e):
                for r in ops['dve']:
                    r(e)

            @blk.gpsimd
            def _(e):
                for r in ops['pool']:
                    r(e)

            @blk.sync
            def _(e):
                for r in ops['sp']:
                    r(e)


def split_parts(n, mx):
    k = -(-n // mx)
    base = n // k
    rem = n % k
    out = []
    s = 0
    for i in range(k):
        sz = base + (1 if i < rem else 0)
        out.append((s, sz))
        s += sz
    return out


def build(nseq=NSEQ, stop=None):
    nc = bass.Bass("TRN2", target_bir_lowering=False)

    def din(name, shape, dt=F32):
        return nc.dram_tensor(name, list(shape), dt, kind="ExternalInput").ap()

    xT_d = din('xT', [nseq, 8, 128, S])
    win_d = din('w_in', [2, 52, 128, 1024])
    bp_d = din('bp', [2, 4, 8, 128, 256])
    wout_d = din('w_out', [2, 8, 128, 1024])
    dw1_d = din('dw1', [22, 128, 1024])
    dw3_d = din('dw3', [22, 128, 1024])
    dw2_d = din('dw2', [8, 128, 22 * 128])
    mw1_d = din('mw1', [8, 28, 128, 1024])
    mw3_d = din('mw3', [8, 28, 128, 1024])
    mw2_d = din('mw2', [8, 8, 128, 28 * 128])
    router_d = din('router', [128, 64])
    cols_d = din('cols', [128, NCOL])
    bias_d = din('biasT', [8, 128, 256])
    cmask_d = din('cmask', [128, 128])
    ident_d = din('ident', [128, 128])
    bones_d = din('bones', [128, 128])
    rc_d = din('rc', [128, 32])
    poolbd_d = din('poolbd', [2, 128, 256])
    dlam_d = din('dlam', [2, 128, 128])
    kblk_d = din('kblk', [8, S], BF16)
    out_d = nc.dram_tensor('outT', [nseq, 8, 128, S], F32, kind="ExternalOutput").ap()

    def dscr(name, shape):
        return nc.dram_tensor(name, list(shape), BF16, kind="Internal").ap()
    FAM = {
        'win': (win_d, dscr('sc_win', [2, 52, 128, 1024])),
        'bp': (bp_d, dscr('sc_bp', [2, 4, 8, 128, 256])),
        'wout': (wout_d, dscr('sc_wout', [2, 8, 128, 1024])),
        'dw1': (dw1_d, dscr('sc_dw1', [22, 128, 1024])),
        'dw3': (dw3_d, dscr('sc_dw3', [22, 128, 1024])),
        'dw2': (dw2_d, dscr('sc_dw2', [8, 128, 22 * 128])),
        'mw1': (mw1_d, dscr('sc_mw1', [8, 28, 128, 1024])),
        'mw3': (mw3_d, dscr('sc_mw3', [8, 28, 128, 1024])),
        'mw2': (mw2_d, dscr('sc_mw2', [8, 8, 128, 28 * 128])),
    }

    with ExitStack() as es:
        c = Ctx(nc, es)

        def sb(name, shape, dt):
            return es.enter_context(nc.sbuf_tensor(name, list(shape), dt))

        xT = sb('xTs', [128, 8, S], F32)
        hT = sb('hTs', [128, 8, S], BF16)
        arena = sb('arena', [128, 16 * S], BF16)
        arena32 = arena.bitcast(F32)
        stage = sb('stage', [128, NS_STAGE, 1024], F32)
        wbf = sb('wbf', [128, NW_RING, 1024], BF16)
        vaug = sb('vaug', [128, 16, 192], BF16)
        biasT = sb('biasTs', [128, 2, 256], F32)
        Ebuf = sb('Ebuf', [128, 4, 512], BF16)
        tmpn = sb('tmpn', [128, 2, 256], F32)
        sig = sb('sig', [128, 2, 512], F32)
        rden = sb('rden', [128, 2, 512], F32)
        cols = sb('colss', [128, NCOL], F32)
        ident = sb('idents', [128, 128], F32)
        ones32 = sb('ones32', [128, 128], F32)
        bones = sb('boness', [128, 128], F32)
        cmask = sb('cmasks', [128, 128], F32)
        rc = sb('rcs', [128, 32], F32)
        poolbd = sb('poolbds', [128, 2, 256], BF16)
        dlam = sb('dlams', [128, 2, 128], F32)
        lamw = sb('lamw', [128, 16], F32)
        ksum = sb('ksum', [128, 8], F32)
        gsb = sb('gsb', [128, 2, 8, 8], F32)
        gm = sb('gm', [128, 2, 8, 8], F32)
        mbt = sb('mbt', [128, 2, 8, 8], F32)
        m8 = sb('m8', [128, 16, 8], F32)
        router32 = sb('router32', [128, 64], F32)
        lg = sb('lg', [128, 16, 8], F32)
        ex = sb('ex', [128, 16, 8], F32)
        gtok = sb('gtok', [128, 16, 8], F32)
        smallc = sb('smallc', [128, 64], F32)
        grep = dlam
        ps = [es.enter_context(nc.psum_tensor('ps%d' % i, [128, 512], F32)) for i in range(8)]
        PK = ['ps%d' % i for i in range(8)]

        def ct(i):
            return arena[:, i * S:(i + 1) * S]

        def ctk(i):
            return 'ct%d' % i

        def sc32(k):
            return arena32[:, k * S:(k + 1) * S]

        def sck(k):
            return [ctk(2 * k), ctk(2 * k + 1)]

        bc = sc32(7)
        BCK = sck(7)

        def col(i):
            return cols[:, i:i + 1]

        def dma_in(dst, src, key, sem=None):
            return c.op('sp', lambda e: e.dma_start(out=dst, in_=src), writes=[key], dma=(sem or key))

        def mm_group(out, pskey, pairs, rkeys):
            def fn(e, pairs=pairs, out=out):
                n = len(pairs)
                ins = None
                for i, (l, r) in enumerate(pairs):
                    ins = e.matmul(out, l, r, start=(i == 0), stop=(i == n - 1))
                return ins
            return c.op('pe', fn, reads=rkeys, writes=[pskey])

        def mm1(out, pskey, l, r, start, stop, rkeys):
            return c.op('pe', lambda e: e.matmul(out, l, r, start=start, stop=stop), reads=rkeys, writes=[pskey])

        wstate = {'s': 0, 'w': 0}
        wcached = set()

        def flush_store():
            p = wstate.pop('pending', None)
            if p is not None:
                scr, wdst, wk, ckey, wi = p
                c.op('sp', lambda e: e.dma_start(out=scr, in_=wdst), reads=[wk], writes=[ckey], dma='wst%d' % wi)

        def wload(fam, idx, kc, cs=None):
            f32d, bfd = FAM[fam]
            src = f32d[idx]
            scr = bfd[idx]
            if cs is not None:
                src = src[:, cs]
                scr = scr[:, cs]
            wi = wstate['w'] % NW_RING
            wstate['w'] += 1
            wk = 'wbf%d' % wi
            wdst = wbf[:, wi, 0:kc * 128]
            ckey = 'wsc:%s:%s:%s' % (fam, idx, cs)
            if ckey in wcached:
                flush_store()
                c.op('sp', lambda e: e.dma_start(out=wdst, in_=scr), reads=[ckey], writes=[wk], dma='wld%d' % wi)
            else:
                wcached.add(ckey)
                si = wstate['s'] % NS_STAGE
                wstate['s'] += 1
                sk = 'stg%d' % si
                sdst = stage[:, si, 0:kc * 128]
                c.op('sp', lambda e: e.dma_start(out=sdst, in_=src), writes=[sk], dma=sk)
                flush_store()
                c.op('pool', lambda e: e.tensor_copy(wdst, sdst), reads=[sk], writes=[wk])
                wstate['pending'] = (scr, wdst, wk, ckey, wi)
            return wbf[:, wi, 0:kc * 128].rearrange("p (k n) -> p k n", n=128), wk

        rot = {}

        def nxt(name, n):
            v = rot.get(name, 0)
            rot[name] = v + 1
            return v % n

        dma_in(cols[:], cols_d, 'cols')
        dma_in(ident[:], ident_d, 'ident')
        dma_in(bones[:], bones_d, 'bones')
        dma_in(cmask[:], cmask_d, 'cmask')
        dma_in(rc[:], rc_d, 'rc')
        dma_in(router32[:], router_d, 'router32')
        dma_in(dlam[:, 0, :], dlam_d[0], 'grep0')
        dma_in(dlam[:, 1, :], dlam_d[1], 'grep1')
        for l in range(2):
            dma_in(stage[:, 0, 0:256], poolbd_d[l], 'stg0', sem='stg0')
            c.op('pool', lambda e, l=l: e.tensor_copy(poolbd[:, l, :], stage[:, 0, 0:256]), reads=['stg0'], writes=['poolbd'])
        c.op('dve', lambda e: e.memset(ones32[:], 1.0), writes=['ones32'])
        c.op('dve', lambda e: e.memset(vaug[:], 1.0), writes=['vaug'])
        c.op('dve', lambda e: e.memset(mbt[:], 0.0), writes=['mbt'])
        c.op('dve', lambda e: e.memset(gm[:], -1e30), writes=['gm'])
        lam_inits = [0.8 - 0.6 * math.exp(-0.3 * l) for l in range(2)]
        for l in range(2):
            dl = dlam[:, l, :].rearrange("p (a b) -> p a b", b=32)
            c.op('dve', lambda e, dl=dl: e.tensor_tensor(smallc[:, 0:32], dl[:, 0, :], dl[:, 1, :], ALU.mult), reads=['grep%d' % l], writes=['smallc'])
            c.op('dve', lambda e, l=l: e.reduce_sum(lamw[:, 8 + 2 * l:9 + 2 * l], smallc[:, 0:32], AX.X), reads=['smallc'], writes=['lamw'])
            c.op('dve', lambda e, dl=dl: e.tensor_tensor(smallc[:, 0:32], dl[:, 2, :], dl[:, 3, :], ALU.mult), reads=['grep%d' % l, 'lamw'], writes=['smallc'])
            c.op('dve', lambda e, l=l: e.reduce_sum(lamw[:, 9 + 2 * l:10 + 2 * l], smallc[:, 0:32], AX.X), reads=['smallc'], writes=['lamw'])
            c.op('act', lambda e, l=l: e.activation(lamw[:, 12 + 2 * l:14 + 2 * l], lamw[:, 8 + 2 * l:10 + 2 * l], AF.Exp), reads=['lamw'], writes=['lamw'])
            c.op('dve', lambda e, l=l: e.scalar_tensor_tensor(lamw[:, l:l + 1], lamw[:, 13 + 2 * l:14 + 2 * l], -lam_inits[l],
                                                              lamw[:, 12 + 2 * l:13 + 2 * l], ALU.add, ALU.subtract), reads=['lamw'], writes=['lamw'])
            c.op('dve', lambda e, l=l: e.tensor_scalar(lamw[:, 4 + l:5 + l], col(C_SUBG + l), 1.0 - lam_inits[l], None, ALU.mult), reads=['cols', 'lamw'], writes=['lamw'])

        def norm_group(g):
            gs = slice(g * 512, (g + 1) * 512)
            pb = 6 + nxt('nps', 2)
            for kc in range(8):
                sq = nxt('sig', 2)
                c.op('act', lambda e, kc=kc, sq=sq: e.activation(sig[:, sq, :], xT[:, kc, gs], AF.Square),
                     reads=['xT%d' % kc], writes=['sig%d' % sq])
                mm1(ps[pb][:, :], PK[pb], ones32[:], sig[:, sq, :], kc == 0, kc == 7, ['ones32', 'sig%d' % sq])
            c.op('dve', lambda e: e.tensor_scalar(bc[:, gs], ps[pb][:, :], 1.0 / D, 1e-6, ALU.mult, ALU.add),
                 reads=[], writes=[PK[pb]] + BCK)
            c.op('act', lambda e: e.sqrt(bc[:, gs], bc[:, gs]), reads=[], writes=BCK)
            c.op('dve', lambda e: e.reciprocal(bc[:, gs], bc[:, gs]), reads=[], writes=BCK)

        def norm_stats():
            for g in range(4):
                norm_group(g)

        def build_h_group(gbase, g):
            gs = slice(g * 512, (g + 1) * 512)
            for kc in range(8):
                c.op('dve', lambda e, kc=kc: e.scalar_tensor_tensor(hT[:, kc, gs], xT[:, kc, gs], col(gbase + kc), bc[:, gs], ALU.mult, ALU.mult),
                     reads=['xT%d' % kc, 'cols'] + BCK, writes=['hT%d_%d' % (kc, g)])

        def build_h(gbase):
            for kc in range(8):
                eng = 'dve'
                c.op(eng, lambda e, kc=kc: e.scalar_tensor_tensor(hT[:, kc, :], xT[:, kc, :], col(gbase + kc), bc, ALU.mult, ALU.mult),
                     reads=['xT%d' % kc, 'cols'] + BCK, writes=['hT%d_%d' % (kc, g) for g in range(4)])

        def HKg(g):
            return ['hT%d_%d' % (k, g) for k in range(8)]

        HK = ['hT%d_%d' % (k, g) for k in range(8) for g in range(4)]

        def proj_fm(l, blk, evac):
            w, wk = wload('win', (l, blk), 8)
            for g in range(4):
                pb = 6 + nxt('pps', 2)
                gs = slice(g * 512, (g + 1) * 512)
                mm_group(ps[pb][:, :], PK[pb], [(w[:, kc, :], hT[:, kc, gs]) for kc in range(8)], HKg(g) + [wk])
                evac(g, pb)

        def proj_tm(l, blk, evac4):
            w, wk = wload('win', (l, blk), 8)
            for g in range(4):
                pb = 6 + nxt('pps', 2)
                for j in range(4):
                    kt = g * 4 + j
                    mm_group(ps[pb][:, j * 128:(j + 1) * 128], PK[pb],
                             [(hT[:, kc, kt * 128:(kt + 1) * 128], w[:, kc, :]) for kc in range(8)], HKg(g) + [wk])
                evac4(g, pb)

        def attention(units, jj, vsl, bias_j, farcol, obanks, finalize, LA=2):
            steps = []
            for QG in range(4):
                nk = 4 * QG + 4
                for kt in range(nk):
                    for m in range(len(units)):
                        steps.append((QG, kt, m, nk))

            def emit_S(QG, kt, m, nk):
                kfn, qfn, rk = units[m]
                q0 = max(QG * 512, kt * 128)
                n = (QG + 1) * 512 - q0
                col0 = q0 - QG * 512
                sbk = (0, 1, 2, 7)[nxt('sps', 4)]
                mm_group(ps[sbk][:, 0:n], PK[sbk], [(kfn(kt), qfn(q0, n))], rk)
                eb = nxt('E', 4)
                ek = 'E%d' % eb
                if q0 == kt * 128:
                    nn = min(2, n // 128)
                    bsl = biasT[:, bias_j, 0:nn * 128]
                elif q0 == (kt + 1) * 128:
                    nn = 1
                    bsl = biasT[:, bias_j, 128:256]
                else:
                    nn = 0
                    bsl = None
                if nn > 0:
                    tb = nxt('tmpn', 2)
                    c.op('dve', lambda e: e.tensor_tensor(tmpn[:, tb, 0:nn * 128], ps[sbk][:, 0:nn * 128], bsl, ALU.add),
                         reads=['biasT%d' % bias_j], writes=[PK[sbk], 'tmpn%d' % tb])
                    c.op('act', lambda e: e.activation(Ebuf[:, eb, 0:nn * 128], tmpn[:, tb, 0:nn * 128], AF.Exp),
                         reads=['tmpn%d' % tb], writes=[ek])
                if n > nn * 128:
                    c.op('act', lambda e: e.activation(Ebuf[:, eb, nn * 128:n], ps[sbk][:, nn * 128:n], AF.Exp, bias=farcol, scale=1.0),
                         reads=['cols'], writes=[PK[sbk], ek])
                return (QG, kt, m, nk, eb, n, col0)

            def emit_PV(QG, kt, m, nk, eb, n, col0):
                ob = obanks[QG % 2]
                mm1(ps[ob[m]][:, col0:col0 + n], PK[ob[m]], vaug[:, kt, vsl], Ebuf[:, eb, 0:n], kt == 0, kt == nk - 1,
                    ['vaug', 'E%d' % eb])
                if kt == nk - 1 and m == len(units) - 1:
                    finalize(QG, ob)

            pend = []
            for i in range(len(steps) + LA):
                if i < len(steps):
                    pend.append(emit_S(*steps[i]))
                if i >= LA:
                    emit_PV(*pend[i - LA])

        def load_bias(h0):
            for j in range(2):
                dma_in(biasT[:, j, :], bias_d[h0 + j], 'biasT%d' % j)
                c.op('dve', lambda e, j=j: e.tensor_tensor(biasT[:, j, 0:128], biasT[:, j, 0:128], cmask[:], ALU.add),
                     reads=['cmask'], writes=['biasT%d' % j])

        def v_proj(l, blk):
            def evac4(g, pb):
                pv = ps[pb][:, :].rearrange("p (a h d) -> p a h d", a=4, h=2)
                c.op('dve', lambda e, g=g, pv=pv: e.tensor_copy(vaug[:, 4 * g:4 * g + 4, 0:64], pv[:, :, 0, :]),
                     reads=[], writes=[PK[pb], 'vaug'])
                c.op('act', lambda e, g=g, pv=pv: e.copy(vaug[:, 4 * g:4 * g + 4, 128:192], pv[:, :, 1, :]),
                     reads=[], writes=[PK[pb], 'vaug'])
            proj_tm(l, blk, evac4)

        QA, QB, KA, KB = 8, 9, 10, 11

        def moba_pair(l, cpair):
            q32 = sc32(6)
            q32k = sck(6)
            load_bias(2 * cpair)

            def evac_q(g, pb):
                gs = slice(g * 512, (g + 1) * 512)
                c.op('act', lambda e: e.mul(ct(QA)[0:64, gs], ps[pb][0:64, :], 0.125), writes=[PK[pb], ctk(QA)])
                c.op('act', lambda e: e.mul(ct(QB)[0:64, gs], ps[pb][64:128, :], 0.125), writes=[PK[pb], ctk(QB)])
                c.op('dve', lambda e: e.tensor_scalar(q32[:, gs], ps[pb][:, :], 0.125, None, ALU.mult), writes=[PK[pb]] + q32k)
            proj_fm(l, cpair, evac_q)

            def evac_k(g, pb):
                gs = slice(g * 512, (g + 1) * 512)
                c.op('act', lambda e: e.copy(ct(KA)[0:64, gs], ps[pb][0:64, :]), writes=[PK[pb], ctk(KA)])
                c.op('act', lambda e: e.copy(ct(KB)[0:64, gs], ps[pb][64:128, :]), writes=[PK[pb], ctk(KB)])
                c.op('dve', lambda e: e.tensor_reduce(ksum[:, 2 * g:2 * g + 2], ps[pb][:, :].rearrange("p (a b) -> p a b", b=256), AX.X, ALU.add),
                     writes=[PK[pb], 'ksum'])
            proj_fm(l, 2 + cpair, evac_k)
            for t in (KA, KB):
                c.op('dve', lambda e, t=t: e.memset(ct(t)[64:128, :], 0.0), writes=[ctk(t)])
                dma_in(ct(t)[64:72, :], kblk_d, ctk(t), sem='kb%d' % t)
            for t in (QA, QB):
                c.op('dve', lambda e, t=t: e.memset(ct(t)[64:128, :], 0.0), writes=[ctk(t)])
            c.op('dve', lambda e: e.tensor_scalar(ksum[:, :], ksum[:, :], 1.0 / 256, None, ALU.mult), writes=['ksum'])
            for j in range(2):
                hs = slice(64 * j, 64 * j + 64)
                for qt in range(8, 16):
                    mm_group(ps[5][:, j * 128 + qt * 8:j * 128 + qt * 8 + 8], PK[5],
                             [(q32[hs, qt * 128:(qt + 1) * 128], ksum[hs, 0:8])], q32k + ['ksum'])
            c.op('dve', lambda e: e.tensor_copy(gsb[:, :, 0:8, :], ps[5][:, 0:256].rearrange("p (j q b) -> p j q b", j=2, q=16)[:, :, 8:16, :]),
                 writes=[PK[5], 'gsb'])
            v_proj(l, 4 + cpair)
            for own in range(4, 8):
                c.op('dve', lambda e, own=own: e.tensor_copy(gm[:, :, 2 * own - 8:2 * own - 6, 0:own], gsb[:, :, 2 * own - 8:2 * own - 6, 0:own]),
                     reads=['gsb'], writes=['gm'])
            for j in range(2):
                qt_tile = QA if j == 0 else QB
                for qt in range(8, 16):
                    own = qt // 2
                    c.op('dve', lambda e, j=j, qt=qt: e.max(m8[:, qt, :], gm[:, j, qt - 8, :]), reads=['gm'], writes=['m8_%d' % qt])
                    c.op('dve', lambda e, j=j, qt=qt, own=own: e.tensor_scalar(mbt[:, j, qt - 8, 0:own], gm[:, j, qt - 8, 0:own], m8[:, qt, 2:3], NEG,
                                                                             ALU.is_lt, ALU.mult),
                         reads=['gm', 'm8_%d' % qt], writes=['mbt'])
                for half in range(2):
                    for i in range(4):
                        qt = 8 + half * 4 + i
                        c.op('pe', lambda e, j=j, qt=qt, i=i: e.transpose(ps[5][0:8, i * 128:(i + 1) * 128], mbt[:, j, qt - 8, :], ident[:]),
                             reads=['mbt', 'ident'], writes=[PK[5]])
                    c.op('act', lambda e, half=half, qt_tile=qt_tile: e.copy(
                        ct(qt_tile)[64:72, 1024 + half * 512:1536 + half * 512], ps[5][0:8, :]),
                        writes=[PK[5], ctk(qt_tile)])
            for j in range(2):
                qt_tile, kt_tile = (QA, KA) if j == 0 else (QB, KB)
                units = [(lambda kt, kt_tile=kt_tile: ct(kt_tile)[:, kt * 128:(kt + 1) * 128],
                          lambda q0, n, qt_tile=qt_tile: ct(qt_tile)[:, q0:q0 + n],
                          [ctk(qt_tile), ctk(kt_tile)])]
                vsl = slice(0, 128) if j == 0 else slice(64, 192)
                num = slice(64 * j, 64 * j + 64)
                den = slice(64 * (1 - j), 64 * (1 - j) + 64)

                def fin(QG, ob, num=num, den=den):
                    gs = slice(QG * 512, (QG + 1) * 512)
                    rb = nxt('rden', 2)
                    c.op('dve', lambda e: e.reciprocal(rden[num, rb, :], ps[ob[0]][den, :]), writes=[PK[ob[0]], 'rden%d' % rb])
                    c.op('dve', lambda e: e.tensor_tensor(ct(cpair)[num, gs], ps[ob[0]][num, :], rden[num, rb, :], ALU.mult),
                         reads=['rden%d' % rb], writes=[PK[ob[0]], ctk(cpair)])
                attention(units, j, vsl, j, col(C_FAR + 2 * cpair + j), [(3,), (4,)], fin)

        def diff_pair(l, cpair):
            osc = sc32(7)
            osck = sck(7)
            load_bias(4 + 2 * cpair)
            qs = 32 ** -0.5

            def evac_q(g, pb):
                gs = slice(g * 512, (g + 1) * 512)
                if g % 2 == 0:
                    c.op('act', lambda e: e.mul(ct(QA)[:, gs], ps[pb][:, :], qs), writes=[PK[pb], ctk(QA)])
                else:
                    c.op('dve', lambda e: e.tensor_scalar(ct(QA)[:, gs], ps[pb][:, :], qs, None, ALU.mult), writes=[PK[pb], ctk(QA)])
            proj_fm(l, 14 + cpair, evac_q)

            KT = {(0, 0): 10, (0, 1): 11, (1, 0): 12, (1, 1): 13}

            def evac_k(g, pb):
                gs = slice(g * 512, (g + 1) * 512)
                for j in range(2):
                    for m in range(2):
                        t = KT[(j, m)]
                        mcol = col(C_MSK + 2 * j + m)
                        if m == 0:
                            c.op('act', lambda e, t=t, mcol=mcol: e.mul(ct(t)[:, gs], ps[pb][:, :], mcol), reads=['cols'], writes=[PK[pb], ctk(t)])
                        else:
                            c.op('dve', lambda e, t=t, mcol=mcol: e.tensor_scalar(ct(t)[:, gs], ps[pb][:, :], mcol, None, ALU.mult),
                                 reads=['cols'], writes=[PK[pb], ctk(t)])
            proj_fm(l, 16 + cpair, evac_k)
            v_proj(l, 18 + cpair)
            for j in range(2):
                units = []
                for m in range(2):
                    kt_tile = KT[(j, m)]
                    units.append((lambda kt, kt_tile=kt_tile: ct(kt_tile)[:, kt * 128:(kt + 1) * 128],
                                  lambda q0, n: ct(QA)[:, q0:q0 + n],
                                  [ctk(QA), ctk(kt_tile)]))
                vsl = slice(0, 128) if j == 0 else slice(64, 192)
                num = slice(64 * j, 64 * j + 64)
                den = slice(64 * (1 - j), 64 * (1 - j) + 64)

                def fin(QG, ob, num=num, den=den):
                    gs = slice(QG * 512, (QG + 1) * 512)
                    c.op('dve', lambda e: e.reciprocal(rden[num, 0, :], ps[ob[0]][den, :]), writes=[PK[ob[0]], 'rden0'])
                    c.op('dve', lambda e: e.tensor_tensor(osc[num, gs], ps[ob[0]][num, :], rden[num, 0, :], ALU.mult),
                         reads=['rden0'], writes=[PK[ob[0]]] + osck)
                    c.op('dve', lambda e: e.reciprocal(rden[num, 1, :], ps[ob[1]][den, :]), writes=[PK[ob[1]], 'rden1'])
                    c.op('dve', lambda e: e.tensor_tensor(rden[num, 1, :], ps[ob[1]][num, :], rden[num, 1, :], ALU.mult),
                         writes=[PK[ob[1]], 'rden1'])
                    c.op('dve', lambda e: e.scalar_tensor_tensor(osc[num, gs], rden[num, 1, :], lamw[num, l:l + 1], osc[num, gs], ALU.mult, ALU.add),
                         reads=['rden1', 'lamw'], writes=osck)
                attention(units, j, vsl, j, col(C_FAR + 4 + 2 * cpair + j), [(3, 4), (5, 6)], fin)
            for g in range(4):
                gs = slice(g * 512, (g + 1) * 512)
                sq = nxt('sig', 2)
                c.op('act', lambda e, sq=sq, gs=gs: e.activation(sig[:, sq, :], osc[:, gs], AF.Square), reads=osck, writes=['sig%d' % sq])
                pb = 6 + nxt('pps', 2)
                mm1(ps[pb][:, :], PK[pb], bones[:], sig[:, sq, :], True, True, ['bones', 'sig%d' % sq])
                c.op('dve', lambda e, pb=pb: e.tensor_scalar(rden[:, 0, :], ps[pb][:, :], 1.0 / 64, 1e-5, ALU.mult, ALU.add), writes=[PK[pb], 'rden0'])
                c.op('act', lambda e: e.sqrt(rden[:, 0, :], rden[:, 0, :]), writes=['rden0'])
                c.op('dve', lambda e: e.reciprocal(rden[:, 0, :], rden[:, 0, :]), writes=['rden0'])
                c.op('dve', lambda e, gs=gs: e.scalar_tensor_tensor(ct(6 + cpair)[:, gs], osc[:, gs], lamw[:, 4 + l:5 + l], rden[:, 0, :], ALU.mult, ALU.mult),
                     reads=osck + ['lamw', 'rden0'], writes=[ctk(6 + cpair)])

        def conv_chunk(l, cc):
            XB, BB, CB = 8, 9, 10
            u = sc32(6)
            uk = sck(6)
            acc = sc32(7)
            acck = sck(7)

            def mk_evac(tile):
                def ev(g, pb):
                    gs = slice(g * 512, (g + 1) * 512)
                    eng = 'act' if g % 2 == 0 else 'dve'
                    if eng == 'act':
                        c.op('act', lambda e: e.copy(ct(tile)[:, gs], ps[pb][:, :]), writes=[PK[pb], ctk(tile)])
                    else:
                        c.op('dve', lambda e: e.tensor_copy(ct(tile)[:, gs], ps[pb][:, :]), writes=[PK[pb], ctk(tile)])
                return ev
            proj_fm(l, 6 + cc, mk_evac(XB))
            proj_fm(l, 8 + cc, mk_evac(BB))
            proj_fm(l, 10 + cc, mk_evac(CB))
            w0 = col(C_CONV + (l * 2 + cc) * 3 + 0)
            w1 = col(C_CONV + (l * 2 + cc) * 3 + 1)
            w2 = col(C_CONV + (l * 2 + cc) * 3 + 2)
            c.op('dve', lambda e: e.tensor_tensor(u, ct(CB), ct(XB), ALU.mult), reads=[ctk(CB), ctk(XB)], writes=uk)
            c.op('dve', lambda e: e.tensor_scalar(acc, u, w2, None, ALU.mult), reads=uk + ['cols'], writes=acck)
            c.op('dve', lambda e: e.scalar_tensor_tensor(acc[:, 1:S], u[:, 0:S - 1], w1, acc[:, 1:S], ALU.mult, ALU.add), reads=uk + ['cols'], writes=acck)
            c.op('dve', lambda e: e.scalar_tensor_tensor(acc[:, 2:S], u[:, 0:S - 2], w0, acc[:, 2:S], ALU.mult, ALU.add), reads=uk + ['cols'], writes=acck)
            c.op('dve', lambda e: e.tensor_tensor(ct(2 + cc), acc, ct(BB), ALU.mult), reads=acck + [ctk(BB)], writes=[ctk(2 + cc)])

        def pool_chunk(l, cc):
            u = sc32(4)
            uk = sck(4)
            s1 = sc32(5)
            s1k = sck(5)
            s2 = sc32(6)
            s2k = sck(6)
            PT = 14

            def ev(g, pb):
                gs = slice(g * 512, (g + 1) * 512)
                c.op('act', lambda e: e.copy(u[:, gs], ps[pb][:, :]), writes=[PK[pb]] + uk)
            proj_fm(l, 12 + cc, ev)
            hi = slice(64, 128)
            c.op('dve', lambda e: e.tensor_copy(s1[:, 0:1], u[:, 0:1]), reads=uk, writes=s1k)
            c.op('dve', lambda e: e.tensor_tensor(s1[:, 1:S], u[:, 1:S], u[:, 0:S - 1], ALU.add), reads=uk, writes=s1k)
            if cc == 0:
                c.op('dve', lambda e: e.tensor_copy(s2[hi, 0:2], s1[hi, 0:2]), reads=s1k, writes=s2k)
                c.op('dve', lambda e: e.tensor_tensor(s2[hi, 2:S], s1[hi, 2:S], s1[hi, 0:S - 2], ALU.add), reads=s1k, writes=s2k)
                c.op('dve', lambda e: e.tensor_copy(s2[0:64, :], s1[0:64, :]), reads=s1k, writes=s2k)
                fin = s2
                fink = s2k
                wlo, whi = 2, 4
            else:
                c.op('dve', lambda e: e.tensor_copy(s2[:, 0:2], s1[:, 0:2]), reads=s1k, writes=s2k)
                c.op('dve', lambda e: e.tensor_tensor(s2[:, 2:S], s1[:, 2:S], s1[:, 0:S - 2], ALU.add), reads=s1k, writes=s2k)
                c.op('dve', lambda e: e.tensor_copy(s1[:, 0:4], s2[:, 0:4]), reads=s2k, writes=s1k)
                c.op('dve', lambda e: e.tensor_tensor(s1[:, 4:S], s2[:, 4:S], s2[:, 0:S - 4], ALU.add), reads=s2k, writes=s1k)
                c.op('dve', lambda e: e.tensor_copy(s2[hi, 0:8], s1[hi, 0:8]), reads=s1k, writes=s2k)
                c.op('dve', lambda e: e.tensor_tensor(s2[hi, 8:S], s1[hi, 8:S], s1[hi, 0:S - 8], ALU.add), reads=s1k, writes=s2k)
                c.op('dve', lambda e: e.tensor_copy(s2[0:64, :], s1[0:64, :]), reads=s1k, writes=s2k)
                fin = s2
                fink = s2k
                wlo, whi = 8, 16
            lo = slice(0, 64)
            c.op('dve', lambda e: e.scalar_tensor_tensor(ct(PT)[lo, :], fin[lo, :], 1.0 / wlo, u[lo, :], ALU.mult, ALU.subtract), reads=fink + uk, writes=[ctk(PT)])
            c.op('dve', lambda e: e.scalar_tensor_tensor(ct(PT)[hi, :], fin[hi, :], 1.0 / whi, u[hi, :], ALU.mult, ALU.subtract), reads=fink + uk, writes=[ctk(PT)])
            c.op('dve', lambda e: e.tensor_tensor(fin[:, 0:16], fin[:, 0:16], rc[:, 16 * cc:16 * cc + 16], ALU.mult), reads=['rc'], writes=fink)
            c.op('dve', lambda e: e.tensor_tensor(ct(PT)[:, 0:16], fin[:, 0:16], u[:, 0:16], ALU.subtract), reads=fink + uk, writes=[ctk(PT)])
            for g in range(4):
                gs = slice(g * 512, (g + 1) * 512)
                pb = 6 + nxt('pps', 2)
                mm_group(ps[pb][:, :], PK[pb], [(poolbd[:, l, 128 * cc:128 * cc + 128], ct(PT)[:, gs])], ['poolbd', ctk(PT)])
                c.op('act', lambda e, pb=pb, gs=gs: e.mul(ct(4 + cc)[:, gs], ps[pb][:, :], col(C_PSC + 2 * l + cc)),
                     reads=['cols'], writes=[PK[pb], ctk(4 + cc)])

        def merge_out(l, ffn_gbase):
            MG = 8
            mview = arena[:, MG * S:(MG + 2) * S].rearrange("p (a b) -> p a b", b=512)
            mk = [ctk(MG), ctk(MG + 1)]
            macc = rden
            for g in range(4):
                gs = slice(g * 512, (g + 1) * 512)
                for dc in range(8):
                    for br in range(4):
                        wg, wgk = wload('win', (l, 20 + br * 8 + dc), 8)
                        wb, wbk = wload('bp', (l, br, dc), 2)
                        pg = nxt('mps', 2) * 2
                        mm_group(ps[pg][:, :], PK[pg], [(wg[:, kc, :], hT[:, kc, gs]) for kc in range(8)], HKg(g) + [wgk])
                        mm_group(ps[pg + 1][:, :], PK[pg + 1], [(wb[:, kc, :], ct(2 * br + kc)[:, gs]) for kc in range(2)],
                                 [wbk, ctk(2 * br), ctk(2 * br + 1)])
                        sq = nxt('sig', 2)
                        c.op('act', lambda e, pg=pg, sq=sq: e.activation(sig[:, sq, :], ps[pg][:, :], AF.Sigmoid), writes=[PK[pg], 'sig%d' % sq])
                        if br == 0:
                            c.op('dve', lambda e, pg=pg, sq=sq: e.tensor_tensor(macc[:, 0, :], sig[:, sq, :], ps[pg + 1][:, :], ALU.mult),
                                 reads=['sig%d' % sq], writes=[PK[pg + 1], 'rden0'])
                        else:
                            c.op('dve', lambda e, pg=pg, sq=sq: e.tensor_tensor(macc[:, 1, :], sig[:, sq, :], ps[pg + 1][:, :], ALU.mult),
                                 reads=['sig%d' % sq], writes=[PK[pg + 1], 'rden1'])
                            if br < 3:
                                c.op('dve', lambda e: e.tensor_tensor(macc[:, 0, :], macc[:, 0, :], macc[:, 1, :], ALU.add),
                                     reads=['rden1'], writes=['rden0'])
                            else:
                                c.op('dve', lambda e, dc=dc: e.tensor_tensor(mview[:, dc, :], macc[:, 0, :], macc[:, 1, :], ALU.add),
                                     reads=['rden1', 'rden0'], writes=mk)
                for do in range(8):
                    w, wk = wload('wout', (l, do), 8)
                    pb = 4 + nxt('ops', 2)
                    mm_group(ps[pb][:, :], PK[pb], [(w[:, kc, :], mview[:, kc, :]) for kc in range(8)], mk + [wk])
                    c.op('dve', lambda e, pb=pb, do=do, gs=gs: e.tensor_tensor(xT[:, do, gs], xT[:, do, gs], ps[pb][:, :], ALU.add),
                         writes=[PK[pb], 'xT%d' % do])
                norm_group(g)
                build_h_group(ffn_gbase, g)

        def ffn(experts):
            work = []
            for ei, (w1f, w3f, w2f, nf, ge) in enumerate(experts):
                for pi, (f0, fp) in enumerate(split_parts(nf, 7)):
                    work.append((ei, pi, f0, fp))

            def phaseA(wi):
                ei, pi, f0, fp = work[wi]
                w1f, w3f, w2f, nf, ge = experts[ei]
                aset = (wi % 2) * 7
                if ge is not None and pi == 0:
                    build_gbc(ge)
                for j in range(fp):
                    fi = f0 + j
                    w1, w1k = wload(*w1f(fi))
                    w3, w3k = wload(*w3f(fi))
                    at = aset + j
                    for g in range(4):
                        gs = slice(g * 512, (g + 1) * 512)
                        pb = nxt('fps', 2) * 2
                        mm_group(ps[pb][:, :], PK[pb], [(w1[:, kc, :], hT[:, kc, gs]) for kc in range(8)], HKg(g) + [w1k])
                        mm_group(ps[pb + 1][:, :], PK[pb + 1], [(w3[:, kc, :], hT[:, kc, gs]) for kc in range(8)], HKg(g) + [w3k])
                        sq = nxt('sig', 2)
                        c.op('act', lambda e, pb=pb, sq=sq: e.activation(sig[:, sq, :], ps[pb][:, :], AF.Silu), writes=[PK[pb], 'sig%d' % sq])
                        if ge is None:
                            c.op('dve', lambda e, pb=pb, sq=sq, at=at, gs=gs: e.tensor_tensor(ct(at)[:, gs], sig[:, sq, :], ps[pb + 1][:, :], ALU.mult),
                                 reads=['sig%d' % sq], writes=[PK[pb + 1], ctk(at)])
                        else:
                            c.op('dve', lambda e, pb=pb, sq=sq, gs=gs: e.tensor_tensor(sig[:, sq, :], sig[:, sq, :], ps[pb + 1][:, :], ALU.mult),
                                 writes=[PK[pb + 1], 'sig%d' % sq])
                            c.op('dve', lambda e, sq=sq, at=at, gs=gs: e.tensor_tensor(ct(at)[:, gs], sig[:, sq, :], bc[:, gs], ALU.mult),
                                 reads=['sig%d' % sq] + BCK, writes=[ctk(at)])

            def phaseB(wi):
                ei, pi, f0, fp = work[wi]
                w1f, w3f, w2f, nf, ge = experts[ei]
                aset = (wi % 2) * 7
                for dc in range(8):
                    w2, w2k = wload(*w2f(dc, f0, fp))
                    for g in range(4):
                        gs = slice(g * 512, (g + 1) * 512)
                        pb = 4 + nxt('ops', 2)
                        mm_group(ps[pb][:, :], PK[pb], [(w2[:, j, :], ct(aset + j)[:, gs]) for j in range(fp)],
                                 [w2k] + [ctk(aset + j) for j in range(fp)])
                        c.op('dve', lambda e, pb=pb, dc=dc, gs=gs: e.tensor_tensor(xT[:, dc, gs], xT[:, dc, gs], ps[pb][:, :], ALU.add),
                             writes=[PK[pb], 'xT%d' % dc])
            phaseA(0)
            for wi in range(len(work)):
                if wi + 1 < len(work):
                    phaseA(wi + 1)
                phaseB(wi)

        def build_gbc(ge):
            for g in range(4):
                pb = 6 + nxt('pps', 2)
                for j in range(4):
                    kt = 4 * g + j
                    gb = nxt('grep', 2)
                    c.op('dve', lambda e, gb=gb, kt=kt: e.tensor_copy(grep[:, gb, :], gtok[:, kt, ge:ge + 1].to_broadcast([128, 128])),
                         reads=['gtok'], writes=['grep%d' % gb])
                    mm_group(ps[pb][:, j * 128:(j + 1) * 128], PK[pb], [(grep[:, gb, :], ident[:])], ['grep%d' % gb, 'ident'])
                c.op('act', lambda e, pb=pb, g=g: e.copy(bc[:, g * 512:(g + 1) * 512], ps[pb][:, :]), writes=[PK[pb]] + BCK)

        def router(gbase):
            for g in range(4):
                gs = slice(g * 512, (g + 1) * 512)
                for kc in range(8):
                    sq = nxt('sig', 2)
                    c.op('dve', lambda e, kc=kc, sq=sq, gs=gs: e.scalar_tensor_tensor(sig[:, sq, :], xT[:, kc, gs], col(gbase + kc), bc[:, gs], ALU.mult, ALU.mult),
                         reads=['xT%d' % kc, 'cols'] + BCK, writes=['sig%d' % sq])
                    mm1(ps[6][0:8, :], PK[6], router32[:, kc * 8:kc * 8 + 8], sig[:, sq, :], kc == 0, kc == 7, ['router32', 'sig%d' % sq])
                c.op('act', lambda e: e.copy(rden[0:8, 0, :], ps[6][0:8, :]), writes=[PK[6], 'rden0'])
                for j in range(4):
                    c.op('pe', lambda e, j=j: e.transpose(ps[7][:, j * 8:j * 8 + 8], rden[0:8, 0, j * 128:(j + 1) * 128], ident[0:8, 0:8]),
                         reads=['rden0', 'ident'], writes=[PK[7]])
                c.op('dve', lambda e, g=g: e.tensor_copy(lg[:, 4 * g:4 * g + 4, :], ps[7][:, 0:32].rearrange("p (a b) -> p a b", b=8)),
                     writes=[PK[7], 'lg'])
            for kt in range(16):
                mk_ = 'm8_%d' % kt
                c.op('dve', lambda e, kt=kt: e.max(m8[:, kt, :], lg[:, kt, :]), reads=['lg'], writes=[mk_])
                c.op('dve', lambda e, kt=kt: e.tensor_scalar(smallc[:, kt:kt + 1], m8[:, kt, 0:1], -1.0, None, ALU.mult), reads=[mk_], writes=['smallc'])
                c.op('act', lambda e, kt=kt: e.activation(ex[:, kt, :], lg[:, kt, :], AF.Exp, bias=smallc[:, kt:kt + 1], scale=1.0),
                     reads=['lg', 'smallc'], writes=['ex'])
                c.op('dve', lambda e, kt=kt: e.tensor_scalar(gtok[:, kt, :], lg[:, kt, :], m8[:, kt, 1:2], None, ALU.is_ge), reads=['lg', mk_], writes=['gtok'])
                c.op('dve', lambda e, kt=kt: e.tensor_tensor(ex[:, kt, :], ex[:, kt, :], gtok[:, kt, :], ALU.mult), reads=['gtok'], writes=['ex'])
                c.op('dve', lambda e, kt=kt: e.reduce_sum(smallc[:, 16 + kt:17 + kt], ex[:, kt, :], AX.X), reads=['ex'], writes=['smallc'])
                c.op('dve', lambda e, kt=kt: e.reciprocal(smallc[:, 32 + kt:33 + kt], smallc[:, 16 + kt:17 + kt]), writes=['smallc'])
                c.op('dve', lambda e, kt=kt: e.tensor_scalar(gtok[:, kt, :], ex[:, kt, :], smallc[:, 32 + kt:33 + kt], None, ALU.mult),
                     reads=['ex', 'smallc'], writes=['gtok'])

        XK = ['xT%d' % k for k in range(8)]

        def dump_x(s):
            toks = []
            for kc in range(8):
                toks.append(c.op('sp', lambda e, kc=kc: e.dma_start(out=out_d[s, kc], in_=xT[:, kc, :]), reads=['xT%d' % kc], dma='sx%d' % kc))
            return toks

        def dump_tiles(s, tiles):
            toks = []
            for i, t in enumerate(tiles):
                k = i % 2
                c.op('dve', lambda e, t=t, k=k: e.tensor_copy(sc32(6 + k), ct(t)), reads=[ctk(t)], writes=sck(6 + k))
                toks.append(c.op('sp', lambda e, i=i, k=k: e.dma_start(out=out_d[s, i], in_=sc32(6 + k)), reads=sck(6 + k), dma='sd%d' % k))
            return toks

        out_toks = []
        for s in range(nseq):
            for kc in range(8):
                dma_in(xT[:, kc, :], xT_d[s, kc], 'xT%d' % kc, sem='xld%d' % kc)
            done = False
            for l in range(2):
                norm_stats()
                build_h(C_MIXG + 8 * l)
                if stop == 'h%d' % l:
                    c.op('dve', lambda e: e.tensor_copy(ct(0), hT[:, 0, :]), reads=HK, writes=[ctk(0)])
                    c.op('dve', lambda e: e.tensor_copy(ct(1), hT[:, 7, :]), reads=HK, writes=[ctk(1)])
                    out_toks += dump_tiles(s, [0, 1])
                    done = True
                    break
                for cp in range(2):
                    moba_pair(l, cp)
                if stop == 'a%d' % l:
                    out_toks += dump_tiles(s, [0, 1])
                    done = True
                    break
                for cc in range(2):
                    conv_chunk(l, cc)
                for cc in range(2):
                    pool_chunk(l, cc)
                for cp in range(2):
                    diff_pair(l, cp)
                if stop == 'y%d' % l:
                    out_toks += dump_tiles(s, list(range(8)))
                    done = True
                    break
                merge_out(l, C_FFNG + 8 * l)
                if stop == 'mix%d' % l:
                    out_toks += dump_x(s)
                    done = True
                    break
                if l == 0:
                    ffn([(lambda fi: ('dw1', (fi,), 8), lambda fi: ('dw3', (fi,), 8),
                          lambda dc, f0, fp: ('dw2', (dc,), fp, slice(f0 * 128, (f0 + fp) * 128)), 22, None)])
                else:
                    router(C_FFNG + 8 * l)
                    ffn([(lambda fi, e_=e_: ('mw1', (e_, fi), 8), lambda fi, e_=e_: ('mw3', (e_, fi), 8),
                          lambda dc, f0, fp, e_=e_: ('mw2', (e_, dc), fp, slice(f0 * 128, (f0 + fp) * 128)), 28, e_) for e_ in range(8)])
                if stop == 'ffn%d' % l:
                    out_toks += dump_x(s)
                    done = True
                    break
            if done:
                continue
            norm_stats()
            for kc in range(8):
                k = kc % 4
                eng = 'dve'
                c.op(eng, lambda e, kc=kc, k=k: e.scalar_tensor_tensor(sc32(k), xT[:, kc, :], col(C_FING + kc), bc, ALU.mult, ALU.mult),
                     reads=['xT%d' % kc, 'cols'] + BCK, writes=sck(k))
                out_toks.append(c.op('sp', lambda e, kc=kc, k=k, s=s: e.dma_start(out=out_d[s, kc], in_=sc32(k)), reads=sck(k), dma='st%d' % k))
        flush_store()
        c.final_wait('sp', out_toks)
        c.emit()
    return nc


def _rel_bucket_np(dist):
    n = np.maximum(dist, 0)
    max_exact = 16
    nf = np.maximum(n, max_exact).astype(np.float32)
    large = max_exact + (np.log(nf / np.float32(max_exact)) / np.float32(math.log(128 / max_exact)) * np.float32(16)).astype(np.int32)
    large = np.minimum(large, 31)
    return np.where(n < max_exact, n, large)


def wblocks(W, nb=None):
    K, N = W.shape
    kc = K // 128
    nbk = N // 128
    a = W.reshape(kc, 128, nbk, 128).transpose(2, 1, 0, 3)
    return np.ascontiguousarray(a).reshape(nbk, 128, kc * 128)


def prep_shared(inp):
    f = np.float32
    sh = {}
    sh['w_in'] = np.stack([wblocks(np.asarray(inp['w_in'][l], f)) for l in range(2)])
    bp = np.asarray(inp['branch_proj'], f)
    sh['bp'] = np.stack([np.stack([wblocks(bp[l, b]) for b in range(4)]) for l in range(2)])
    sh['w_out'] = np.stack([wblocks(np.asarray(inp['w_out'][l], f)) for l in range(2)])
    sh['dw1'] = wblocks(np.asarray(inp['dense_w1'][0], f))
    sh['dw3'] = wblocks(np.asarray(inp['dense_w3'][0], f))
    sh['dw2'] = wblocks(np.asarray(inp['dense_w2'][0], f))
    sh['mw1'] = np.stack([wblocks(np.asarray(inp['moe_w1'][0, e], f)) for e in range(8)])
    sh['mw3'] = np.stack([wblocks(np.asarray(inp['moe_w3'][0, e], f)) for e in range(8)])
    sh['mw2'] = np.stack([wblocks(np.asarray(inp['moe_w2'][0, e], f)) for e in range(8)])
    r = np.asarray(inp['moe_router'][0], f)
    sh['router'] = np.ascontiguousarray(r.reshape(8, 128, 8).transpose(1, 0, 2)).reshape(128, 64)
    cols = np.zeros((128, NCOL), f)

    def chunkcols(v):
        return np.asarray(v, f).reshape(-1, 128).T
    for l in range(2):
        cols[:, C_MIXG + 8 * l:C_MIXG + 8 * l + 8] = chunkcols(inp['mix_norm_g'][l])
        cols[:, C_FFNG + 8 * l:C_FFNG + 8 * l + 8] = chunkcols(inp['ffn_norm_g'][l])
        cw = np.asarray(inp['conv_w'][l], f)
        for cc in range(2):
            for i in range(3):
                cols[:, C_CONV + (l * 2 + cc) * 3 + i] = cw[i, cc * 128:(cc + 1) * 128]
        cols[:, C_PSC + 2 * l:C_PSC + 2 * l + 2] = chunkcols(inp['pool_scale'][l])
        sg = np.asarray(inp['diff_subln_g'][l], f)
        cols[:, C_SUBG + l] = np.concatenate([sg, sg])
    cols[:, C_FING:C_FING + 8] = chunkcols(inp['final_norm_g'])
    bt = np.asarray(inp['bias_table'], f)
    for h in range(8):
        cols[:, C_FAR + h] = bt[31, h]
    for u in range(4):
        cols[32 * u:32 * u + 32, C_MSK + u] = 1.0
    sh['cols'] = cols
    s_idx = np.arange(128)[:, None]
    t_idx = np.arange(128)[None, :]
    bd = _rel_bucket_np(t_idx - s_idx)
    bs = _rel_bucket_np(128 + t_idx - s_idx)
    biasT = np.zeros((8, 128, 256), f)
    for h in range(8):
        biasT[h, :, 0:128] = bt[bd, h]
        biasT[h, :, 128:256] = bt[bs, h]
    sh['biasT'] = biasT
    sh['cmask'] = np.where(t_idx >= s_idx, 0.0, -1e30).astype(f)
    sh['ident'] = np.eye(128, dtype=f)
    bones = np.zeros((128, 128), f)
    bones[0:64, 0:64] = 1.0
    bones[64:128, 64:128] = 1.0
    sh['bones'] = bones
    rc = np.zeros((128, 32), f)
    wins = (2, 4, 8, 16)
    for cc in range(2):
        for half in range(2):
            w = wins[2 * cc + half]
            rc[64 * half:64 * half + 64, 16 * cc:16 * cc + 16] = 1.0 / np.minimum(np.arange(1, 17), w).astype(f)
    sh['rc'] = rc
    pw = np.asarray(inp['pool_w'], f)
    pbd = np.zeros((2, 128, 256), f)
    for l in range(2):
        for cc in range(2):
            for half in range(2):
                pbd[l, 64 * half:64 * half + 64, 128 * cc + 64 * half:128 * cc + 64 * half + 64] = pw[l, 2 * cc + half]
    sh['poolbd'] = pbd
    dl = np.asarray(inp['diff_lambda'], f).reshape(2, 1, 128)
    sh['dlam'] = np.ascontiguousarray(np.broadcast_to(dl, (2, 128, 128)))
    kb = np.zeros((8, S), f)
    for b in range(8):
        kb[b, 256 * b:256 * (b + 1)] = 1.0
    sh['kblk'] = kb.astype(ml_dtypes.bfloat16)
    return sh


_CACHE = {}


def kernel(**inputs):
    x = np.asarray(inputs['x'], np.float32)
    sh = prep_shared(inputs)
    if 'nc' not in _CACHE:
        _CACHE['nc'] = build(NSEQ)
    nc = _CACHE['nc']
    in_maps = []
    for core in range(NCORES):
        xs = x[core * NSEQ:(core + 1) * NSEQ]
        xTt = np.ascontiguousarray(xs.transpose(0, 2, 1)).reshape(NSEQ, 8, 128, S)
        m = dict(sh)
        m['xT'] = xTt
        in_maps.append(m)
    res = run_bass_kernel_spmd(nc, in_maps, core_ids=list(range(NCORES)))
    outs = []
    for core in range(NCORES):
        o = np.asarray(res.results[core]['outT']).reshape(NSEQ, 1024, S)
        outs.append(o.transpose(0, 2, 1))
    return np.ascontiguousarray(np.concatenate(outs, axis=0)).astype(np.float32)
```
